# Optimizing a Trainium2 kernel written in Bass

```python
import math
import jax
import jax.numpy as jnp
from jax import lax
import numpy as np

D_MODEL = 1024
BATCH = 8
SEQ = 8192
DEPTH = 1

N_META = 16
CHUNK = 64
META_PAD = CHUNK - N_META
HG_HEADS = 8
HG_DK = 128
HG_DV = 128
ML_HEADS = 4
ML_DK = 128
ML_DV = 256
CONV_W = 4
FG_BIAS_LO = 3.0
FG_BIAS_HI = 6.0
N_EXPERTS = 64
TOP_K = 8
N_GROUPS = 8
TOPK_GROUPS = 4
D_EXPERT = 256
ROUTED_SCALE = 2.5
MOE_BLOCK = 256
DN_ALPHA = (2.0 * DEPTH) ** 0.25
DN_BETA = (8.0 * DEPTH) ** -0.25
EPS = 1e-5

HG_KW = HG_HEADS * HG_DK
HG_VW = HG_HEADS * HG_DV
ML_KW = ML_HEADS * ML_DK
ML_VW = ML_HEADS * ML_DV
IN_SPLITS = (HG_KW, HG_KW, HG_VW, HG_VW, ML_KW, ML_KW, ML_VW, ML_VW, ML_HEADS, ML_HEADS, D_MODEL, D_MODEL)
IN_WIDTH = sum(IN_SPLITS)

kernel_name = 'hybrid_hgrn2_mlstm_moe_block'


def _layer_norm(x, g, b):
    xf = x.astype(jnp.float32)
    xc = xf - jnp.mean(xf, axis=-1, keepdims=True)
    var = jnp.mean(xc * xc, axis=-1, keepdims=True)
    return (xc * lax.rsqrt(var + EPS) * g + b).astype(x.dtype)


def _head_norm(x, heads, g, center):
    b_, l_, w_ = x.shape
    xf = x.astype(jnp.float32).reshape(b_, l_, heads, w_ // heads)
    if center:
        xf = xf - jnp.mean(xf, axis=-1, keepdims=True)
    var = jnp.mean(xf * xf, axis=-1, keepdims=True)
    return (xf * lax.rsqrt(var + EPS)).reshape(b_, l_, w_) * g


def _front_pad(a, value):
    pads = [(0, 0), (META_PAD, 0)] + [(0, 0)] * (a.ndim - 2)
    return jnp.pad(a, pads, constant_values=value)


def _to_chunks(a, heads):
    b_, lp, w_ = a.shape
    return a.reshape(b_, lp // CHUNK, CHUNK, heads, w_ // heads).transpose(1, 0, 3, 2, 4)


def _from_chunks(o):
    n_, b_, h_, c_, d_ = o.shape
    return o.transpose(1, 0, 3, 2, 4).reshape(b_, n_ * c_, h_ * d_)


def _causal_dwconv(x, w, b):
    y = lax.conv_general_dilated(x, w[:, None, :], window_strides=(1,), padding=[(CONV_W - 1, 0)],
                                 dimension_numbers=('NWC', 'WIO', 'NWC'), feature_group_count=x.shape[-1])
    return y + b


def _hgrn2_chunkwise(q, k, v, log_f):
    q, k, v, log_f = (_to_chunks(_front_pad(a.astype(jnp.float32), 0.0), HG_HEADS) for a in (q, k, v, log_f))
    causal = jnp.tril(jnp.ones((CHUNK, CHUNK), dtype=bool))

    def step(state, inp):
        qc, kc, vc, lfc = inp
        b = jnp.cumsum(lfc, axis=2)
        o_inter = jnp.einsum('bhtk,bhkv->bhtv', qc * jnp.exp(b), state)
        rel = b[:, :, :, None, :] - b[:, :, None, :, :]
        decay = jnp.exp(jnp.where(causal[:, :, None], rel, -jnp.inf))
        scores = jnp.einsum('bhtk,bhsk,bhtsk->bhts', qc, kc, decay)
        o = o_inter + jnp.einsum('bhts,bhsv->bhtv', scores, vc)
        b_last = b[:, :, -1:, :]
        state = jnp.exp(b_last[:, :, 0, :])[..., None] * state + jnp.einsum(
            'bhsk,bhsv->bhkv', kc * jnp.exp(b_last - b), vc)
        return state, o

    s0 = jnp.zeros((q.shape[1], HG_HEADS, HG_DK, HG_DV), jnp.float32)
    _, o = lax.scan(step, s0, (q, k, v, log_f))
    return _from_chunks(o)[:, META_PAD:]


def _mlstm_chunkwise(q, k, v, ig, log_f):
    q, k, v = (_to_chunks(_front_pad(a.astype(jnp.float32), 0.0), ML_HEADS) for a in (q, k, v))
    ig = _to_chunks(_front_pad(ig.astype(jnp.float32), -jnp.inf), ML_HEADS)[..., 0]
    log_f = _to_chunks(_front_pad(log_f.astype(jnp.float32), 0.0), ML_HEADS)[..., 0]
    causal = jnp.tril(jnp.ones((CHUNK, CHUNK), dtype=bool))

    def step(carry, inp):
        c_st, n_st, m_st = carry
        qc, kc, vc, igc, lfc = inp
        b = jnp.cumsum(lfc, axis=-1)
        log_intra = jnp.where(causal, b[..., :, None] - b[..., None, :] + igc[..., None, :], -jnp.inf)
        log_inter = b + m_st[..., None]
        m_t = jnp.maximum(log_inter, jnp.max(log_intra, axis=-1))
        w_intra = jnp.exp(log_intra - m_t[..., None])
        w_inter = jnp.exp(log_inter - m_t)
        s = jnp.einsum('bhtd,bhsd->bhts', qc, kc) * w_intra
        num = w_inter[..., None] * jnp.einsum('bhtd,bhdv->bhtv', qc, c_st) + jnp.einsum('bhts,bhsv->bhtv', s, vc)
        den = w_inter * jnp.einsum('bhtd,bhd->bht', qc, n_st) + jnp.sum(s, axis=-1)
        h = num / jnp.maximum(jnp.abs(den), jnp.exp(-m_t))[..., None]
        b_last = b[..., -1]
        log_w = b_last[..., None] - b + igc
        m_new = jnp.maximum(b_last + m_st, jnp.max(log_w, axis=-1))
        w_s = jnp.exp(log_w - m_new[..., None])
        decay = jnp.exp(b_last + m_st - m_new)
        c_st = decay[..., None, None] * c_st + jnp.einsum('bhsd,bhsv->bhdv', kc * w_s[..., None], vc)
        n_st = decay[..., None] * n_st + jnp.einsum('bhsd,bhs->bhd', kc, w_s)
        return (c_st, n_st, m_new), h

    b_ = q.shape[1]
    carry0 = (jnp.zeros((b_, ML_HEADS, ML_DK, ML_DV), jnp.float32),
              jnp.zeros((b_, ML_HEADS, ML_DK), jnp.float32),
              jnp.zeros((b_, ML_HEADS), jnp.float32))
    _, h = lax.scan(step, carry0, (q, k, v, ig, log_f))
    return _from_chunks(h)[:, META_PAD:]


def _hybrid_mixer(h, w_in, lb, hg_norm_g, conv_w, conv_b, ig_bias, fg_bias, ml_norm_g, w_a, w_b, w_out):
    proj = jnp.einsum('bld,de->ble', h, w_in)
    split_at = [int(s) for s in np.cumsum(IN_SPLITS)[:-1]]
    (q_a, f_a, i_a, g_a, q_b, k_b, v_b, o_b, ig_b, fg_b, gate_a, gate_b) = jnp.split(proj, split_at, axis=-1)
    f = lb + (1.0 - lb) * jax.nn.sigmoid(f_a.astype(jnp.float32))
    o_a = _hgrn2_chunkwise(q_a, 1.0 - f, i_a, jnp.log(f))
    y_a = (_head_norm(o_a, HG_HEADS, hg_norm_g, False) * jax.nn.silu(g_a.astype(jnp.float32))).astype(h.dtype)
    qk = jax.nn.silu(_causal_dwconv(jnp.concatenate([q_b, k_b], axis=-1), conv_w, conv_b))
    q_b, k_b = jnp.split(qk, 2, axis=-1)
    h_b = _mlstm_chunkwise(q_b * (ML_DK ** -0.5), k_b, v_b,
                           ig_b.astype(jnp.float32) + ig_bias,
                           jax.nn.log_sigmoid(fg_b.astype(jnp.float32) + fg_bias))
    y_b = (_head_norm(h_b, ML_HEADS, ml_norm_g, True) * jax.nn.sigmoid(o_b.astype(jnp.float32))).astype(h.dtype)
    merged = (jax.nn.sigmoid(gate_a) * jnp.einsum('blv,vd->bld', y_a, w_a)
              + jax.nn.sigmoid(gate_b) * jnp.einsum('blv,vd->bld', y_b, w_b))
    return jnp.einsum('bld,de->ble', merged, w_out)


def _moe_ffn(h, w_router, router_bias, w_gate, w_up, w_down, w_sh_gate, w_sh_up, w_sh_down):
    b_, l_, d_ = h.shape
    n_tok = b_ * l_
    hf = h.reshape(n_tok, d_)
    scores = jax.nn.sigmoid(jnp.einsum('td,de->te', hf, w_router).astype(jnp.float32))
    biased = scores + router_bias.astype(jnp.float32)
    grouped = biased.reshape(n_tok, N_GROUPS, N_EXPERTS // N_GROUPS)
    group_score = jnp.sum(lax.top_k(grouped, 2)[0], axis=-1)
    _, gidx = lax.top_k(group_score, TOPK_GROUPS)
    gmask = jnp.any(gidx[..., None] == jnp.arange(N_GROUPS)[None, None, :], axis=1)
    emask = jnp.repeat(gmask, N_EXPERTS // N_GROUPS, axis=1)
    _, eidx = lax.top_k(jnp.where(emask, biased, -jnp.inf), TOP_K)
    gw = jnp.take_along_axis(scores, eidx, axis=1)
    gw = gw / jnp.sum(gw, axis=-1, keepdims=True) * ROUTED_SCALE
    n_assign = n_tok * TOP_K
    e_flat = eidx.reshape(-1).astype(jnp.int32)
    tok_flat = jnp.arange(n_assign, dtype=jnp.int32) // TOP_K
    w_flat = gw.reshape(-1)
    order = jnp.argsort(e_flat)
    se, stok, sw = e_flat[order], tok_flat[order], w_flat[order]
    counts = jnp.bincount(e_flat, length=N_EXPERTS).astype(jnp.int32)
    starts = jnp.cumsum(counts) - counts
    padded = (counts + MOE_BLOCK - 1) // MOE_BLOCK * MOE_BLOCK
    pends = jnp.cumsum(padded)
    pstarts = pends - padded
    dest = pstarts[se] + jnp.arange(n_assign, dtype=jnp.int32) - starts[se]
    n_blocks = -(-n_assign // MOE_BLOCK) + N_EXPERTS
    n_slots = n_blocks * MOE_BLOCK
    tok_buf = jnp.full((n_slots,), n_tok, jnp.int32).at[dest].set(stok)
    w_buf = jnp.zeros((n_slots,), jnp.float32).at[dest].set(sw)
    blk_start = jnp.arange(n_blocks, dtype=jnp.int32) * MOE_BLOCK
    blk_expert = jnp.minimum(jnp.searchsorted(pends, blk_start, side='right'), N_EXPERTS - 1)
    h_pad = jnp.concatenate([hf, jnp.zeros((1, d_), hf.dtype)], axis=0)

    def step(acc, inp):
        tok, w, e = inp
        xb = h_pad[tok]
        a = jnp.einsum('td,df->tf', xb, w_gate[e])
        u = jnp.einsum('td,df->tf', xb, w_up[e])
        y = jnp.einsum('tf,fd->td', jax.nn.silu(a) * u, w_down[e]) * w[:, None]
        return acc.at[tok].add(y.astype(acc.dtype)), None

    acc0 = jnp.zeros((n_tok + 1, d_), jnp.float32)
    routed, _ = lax.scan(step, acc0, (tok_buf.reshape(n_blocks, MOE_BLOCK),
                                      w_buf.reshape(n_blocks, MOE_BLOCK), blk_expert))
    shared = jnp.einsum('tf,fd->td', jax.nn.silu(jnp.einsum('td,df->tf', hf, w_sh_gate))
                        * jnp.einsum('td,df->tf', hf, w_sh_up), w_sh_down)
    return (routed[:n_tok] + shared).reshape(b_, l_, d_).astype(h.dtype)


def setup_inputs(seed: int = 0) -> dict:
    key = jax.random.key(seed)
    ks = jax.random.split(key, 27)
    nrm = lambda k, shp: jax.random.normal(k, shp, jnp.float32)
    L = DEPTH
    return {
        'x': nrm(ks[0], (BATCH, SEQ, D_MODEL)),
        'meta_tokens': nrm(ks[1], (N_META, D_MODEL)),
        'ln_emb_g': 1.0 + 0.02 * nrm(ks[2], (D_MODEL,)),
        'ln_emb_b': 0.02 * nrm(ks[3], (D_MODEL,)),
        'w_in': nrm(ks[4], (L, D_MODEL, IN_WIDTH)) * D_MODEL ** -0.5,
        'hg_lb_logits': 0.5 * nrm(ks[5], (DEPTH + 1, HG_KW)),
        'hg_norm_g': 1.0 + 0.02 * nrm(ks[6], (L, HG_VW)),
        'ml_conv_w': nrm(ks[7], (L, CONV_W, 2 * ML_KW)) * CONV_W ** -0.5,
        'ml_conv_b': 0.02 * nrm(ks[8], (L, 2 * ML_KW)),
        'ml_ig_bias': 0.1 * nrm(ks[9], (L, ML_HEADS)),
        'ml_fg_bias': jnp.linspace(FG_BIAS_LO, FG_BIAS_HI, ML_HEADS, dtype=jnp.float32)[None, :] + 0.1 * nrm(ks[10], (L, ML_HEADS)),
        'ml_norm_g': 1.0 + 0.02 * nrm(ks[11], (L, ML_VW)),
        'w_branch_a': nrm(ks[12], (L, HG_VW, D_MODEL)) * HG_VW ** -0.5 * DN_BETA,
        'w_branch_b': nrm(ks[13], (L, ML_VW, D_MODEL)) * ML_VW ** -0.5 * DN_BETA,
        'w_out': nrm(ks[14], (L, D_MODEL, D_MODEL)) * D_MODEL ** -0.5 * DN_BETA,
        'ln1_g': 1.0 + 0.02 * nrm(ks[15], (L, D_MODEL)),
        'ln1_b': 0.02 * nrm(ks[16], (L, D_MODEL)),
        'w_router': nrm(ks[17], (L, D_MODEL, N_EXPERTS)) * D_MODEL ** -0.5,
        'router_bias': 0.01 * nrm(ks[18], (L, N_EXPERTS)),
        'w_exp_gate': nrm(ks[19], (L, N_EXPERTS, D_MODEL, D_EXPERT)) * D_MODEL ** -0.5,
        'w_exp_up': nrm(ks[20], (L, N_EXPERTS, D_MODEL, D_EXPERT)) * D_MODEL ** -0.5,
        'w_exp_down': nrm(ks[21], (L, N_EXPERTS, D_EXPERT, D_MODEL)) * D_EXPERT ** -0.5 * DN_BETA,
        'w_sh_gate': nrm(ks[22], (L, D_MODEL, D_EXPERT)) * D_MODEL ** -0.5,
        'w_sh_up': nrm(ks[23], (L, D_MODEL, D_EXPERT)) * D_MODEL ** -0.5,
        'w_sh_down': nrm(ks[24], (L, D_EXPERT, D_MODEL)) * D_EXPERT ** -0.5 * DN_BETA,
        'ln2_g': 1.0 + 0.02 * nrm(ks[25], (L, D_MODEL)),
        'ln2_b': 0.02 * nrm(ks[26], (L, D_MODEL)),
    }


def reference(x, meta_tokens, ln_emb_g, ln_emb_b, w_in, hg_lb_logits, hg_norm_g, ml_conv_w, ml_conv_b,
              ml_ig_bias, ml_fg_bias, ml_norm_g, w_branch_a, w_branch_b, w_out, ln1_g, ln1_b,
              w_router, router_bias, w_exp_gate, w_exp_up, w_exp_down, w_sh_gate, w_sh_up, w_sh_down,
              ln2_g, ln2_b):
    b_ = x.shape[0]
    meta = jnp.broadcast_to(meta_tokens.astype(x.dtype)[None], (b_, N_META, D_MODEL))
    h = _layer_norm(jnp.concatenate([meta, x], axis=1), ln_emb_g, ln_emb_b)
    lower_bounds = jnp.cumsum(jax.nn.softmax(hg_lb_logits.astype(jnp.float32), axis=0), axis=0)
    for layer in range(DEPTH):
        mix = _hybrid_mixer(h, w_in[layer], lower_bounds[layer], hg_norm_g[layer], ml_conv_w[layer],
                            ml_conv_b[layer], ml_ig_bias[layer], ml_fg_bias[layer], ml_norm_g[layer],
                            w_branch_a[layer], w_branch_b[layer], w_out[layer])
        h = _layer_norm(DN_ALPHA * h + mix, ln1_g[layer], ln1_b[layer])
        ffn = _moe_ffn(h, w_router[layer], router_bias[layer], w_exp_gate[layer], w_exp_up[layer],
                       w_exp_down[layer], w_sh_gate[layer], w_sh_up[layer], w_sh_down[layer])
        h = _layer_norm(DN_ALPHA * h + ffn, ln2_g[layer], ln2_b[layer])
    return h[:, N_META:]
```

```python
import numpy as np
from contextlib import ExitStack
import ml_dtypes
import concourse.bass as bass
import concourse.mybir as mybir
from concourse.bass_utils import run_bass_kernel_spmd

F32 = mybir.dt.float32
BF16 = mybir.dt.bfloat16
AF = mybir.ActivationFunctionType
ALU = mybir.AluOpType

D = 1024
SEQ = 8192
NMETA = 16
TB = 512
CH = 64
NCH = TB // CH
NBLK = 17
EPS = 1e-5
ALPHA = 2.0 ** 0.25
IN_W = 9224
NE = 64
BIG = 1.0e30
import os
RATIO = float(os.environ.get("KRATIO", "0.8"))
HEAD_START = int(os.environ.get("KHEAD", "40"))

C_ID = 0
C_MASK = 128
C_ONES = 192
C_NH = 320
C_SEL = 321
C_RB = 833
C_ZERO = 897
NCST = 961
P_LB0, P_LB1, P_HGG, P_CW, P_CB, P_MLG, P_IGB, P_FGB = 0, 8, 16, 24, 56, 64, 72, 73
NPRM = 74


class Sem:
    def __init__(self, h, name):
        self.h = h
        self.name = name
        self.val = 0


class T:
    __slots__ = ("name", "w", "r", "excl")

    def __init__(self, name=""):
        self.name = name
        self.w = None
        self.r = {}
        self.excl = False


class Eng:
    def __init__(self, name, sem):
        self.name = name
        self.sem = sem
        self.seen = {}
        self.ops = []


class FW:
    def __init__(self, nc, stack):
        self.nc = nc
        self.stack = stack
        self.sems = []
        self.engs = {}
        for n in ("pe", "act", "dve", "pool", "sp"):
            self.engs[n] = Eng(n, self.new_sem("ev_" + n))
        self.n_ops = 0

    def new_sem(self, name):
        h = self.stack.enter_context(self.nc.semaphore(name))
        s = Sem(h, name)
        self.sems.append(s)
        return s

    def _collect(self, e, reads, writes):
        need = {}
        if any(t.excl for t in reads):
            writes = list(writes) + [t for t in reads if t.excl]
            reads = [t for t in reads if not t.excl]

        def add(ev, same_ok):
            if ev is None:
                return
            s, v = ev
            if same_ok and s is e.sem and e.name == "pe":
                return
            if e.seen.get(s, 0) >= v:
                return
            if need.get(s, 0) < v:
                need[s] = v

        for t in reads:
            add(t.w, False)
        for t in writes:
            add(t.w, True)
            for s, v in t.r.items():
                add((s, v), True)
        for s, v in need.items():
            e.seen[s] = v
        return [(s.h, v) for s, v in need.items()]

    def _commit(self, ev, reads, writes):
        s, v = ev
        if any(t.excl for t in reads):
            writes = list(writes) + [t for t in reads if t.excl]
            reads = [t for t in reads if not t.excl]
        for t in reads:
            if t.r.get(s, 0) < v:
                t.r[s] = v
        for t in writes:
            t.w = ev
            t.r = {}

    def op(self, eng, fn, reads=(), writes=()):
        e = self.engs[eng]
        waits = self._collect(e, reads, writes)
        e.sem.val += 1
        ev = (e.sem, e.sem.val)
        semh = e.sem.h

        m, a, kw = fn

        def run(h):
            for sh, v in waits:
                h.wait_ge(sh, v)
            getattr(h, m)(*a, **kw).then_inc(semh, 1)
        e.ops.append(run)
        self._commit(ev, reads, writes)
        self.n_ops += 1

    def group(self, eng, fns, reads=(), writes=()):
        e = self.engs[eng]
        waits = self._collect(e, reads, writes)
        e.sem.val += 1
        ev = (e.sem, e.sem.val)
        semh = e.sem.h

        def run(h):
            for sh, v in waits:
                h.wait_ge(sh, v)
            for (m, a, kw) in fns[:-1]:
                getattr(h, m)(*a, **kw)
            m, a, kw = fns[-1]
            getattr(h, m)(*a, **kw).then_inc(semh, 1)
        e.ops.append(run)
        self._commit(ev, reads, writes)
        self.n_ops += len(fns)

    def dma(self, q, sem, out, in_, reads=(), writes=()):
        e = self.engs[q]
        waits = self._collect(e, reads, writes)
        sem.val += 16
        ev = (sem, sem.val)
        semh = sem.h

        def run(h):
            for sh, v in waits:
                h.wait_ge(sh, v)
            h.dma_start(out=out, in_=in_).then_inc(semh, 16)
        e.ops.append(run)
        self._commit(ev, reads, writes)
        self.n_ops += 1

    def barrier(self):
        for e in self.engs.values():
            lst = []
            for s in self.sems:
                if s.val > 0 and e.seen.get(s, 0) < s.val:
                    lst.append((s.h, s.val))
                    e.seen[s] = s.val

            def run(h, lst=lst):
                for sh, v in lst:
                    h.wait_ge(sh, v)
            e.ops.append(run)

    def emit(self):
        nc = self.nc
        engs = self.engs
        with nc.Block() as block:
            @block.sync
            def _(h):
                for f in engs["sp"].ops:
                    f(h)

            @block.tensor
            def _(h):
                for f in engs["pe"].ops:
                    f(h)

            @block.scalar
            def _(h):
                for f in engs["act"].ops:
                    f(h)

            @block.vector
            def _(h):
                for f in engs["dve"].ops:
                    f(h)

            @block.gpsimd
            def _(h):
                for f in engs["pool"].ops:
                    f(h)
        for e in engs.values():
            e.ops = []


class Buf:
    def __init__(self, t, nT=1, name=""):
        self.t = t
        self.Ts = [T(name + str(i)) for i in range(nT)]

    @property
    def T(self):
        return self.Ts[0]


def w_in_col0(j):
    if j < 56:
        return 128 * j
    if j == 56:
        return 7168
    if j < 65:
        return 7176 + 128 * (j - 57)
    return 8200 + 128 * (j - 65)


class _Stop(Exception):
    pass


def build(NB=NBLK, taps=(), stop=None):
    nc = bass.Bass("TRN2", target_bir_lowering=False)
    NT = NB * TB
    dram = lambda n, shp, dt=F32, kind="ExternalInput": nc.dram_tensor(n, shp, dt, kind=kind).ap()
    xin = dram("xin", [NT, D])
    w_in = dram("w_in", [D, IN_W])
    w_a = dram("w_a", [D, D])
    w_b = dram("w_b", [D, D])
    w_o = dram("w_o", [D, D])
    w_r = dram("w_r", [D, NE])
    w_g = dram("w_g", [NE + 1, D, 256])
    w_u = dram("w_u", [NE + 1, D, 256])
    w_d = dram("w_d", [NE + 1, 256, D])
    cst_d = dram("cst", [128, NCST])
    prm_d = dram("prm", [128, NPRM])
    lnp_d = dram("lnp", [3, 128, 2 * D])
    y = dram("y", [NT, D], F32, "ExternalOutput")
    win_s = dram("win_s", [73, 128, 8 * 128], BF16, "Internal")
    wa_s = dram("wa_s", [8, 128, 8 * 128], BF16, "Internal")
    wb_s = dram("wb_s", [8, 128, 8 * 128], BF16, "Internal")
    wo_s = dram("wo_s", [8, 128, 8 * 128], BF16, "Internal")
    gu_s = dram("gu_s", [NE + 1, 128, 8 * 512], BF16, "Internal")
    dn_s = dram("dn_s", [NE + 1, 128, 2 * 1024], BF16, "Internal")
    tap_out = {}
    for name, shp in taps:
        tap_out[name] = dram("tap_" + name, list(shp), F32, "ExternalOutput")

    with ExitStack() as st:
        fw = FW(nc, st)
        V = lambda fn, r=(), w=(): fw.op("dve", fn, r, w)
        A = lambda fn, r=(), w=(): fw.op("act", fn, r, w)
        G = lambda fn, r=(), w=(): fw.op("pool", fn, r, w)
        MM = lambda fns, r=(), w=(): fw.group("pe", fns, r, w)

        class _Rec:
            def __getattr__(self, m):
                return lambda *a, **kw: (m, a, kw)
        I = _Rec()

        with ExitStack() as pst:
            NS = 8
            stg32 = [pst.enter_context(nc.sbuf_tensor("stg32_%d" % i, [128, 4096], F32)) for i in range(NS)]
            stg16 = [pst.enter_context(nc.sbuf_tensor("stg16_%d" % i, [128, 4096], BF16)) for i in range(NS)]
            T32 = [T() for _ in range(NS)]
            T16 = [T() for _ in range(NS)]
            sl = [fw.new_sem("pl%d" % i) for i in range(NS)]
            ss = [fw.new_sem("ps%d" % i) for i in range(NS)]
            jobs = []

            def add_job(src, dst, a, b):
                jobs.append((src, dst, a, b))

            for j in range(73):
                c0 = w_in_col0(j)
                wd = 8 if j == 56 else 128
                src = w_in[:, c0:c0 + wd].rearrange("(c p) n -> p c n", p=128)
                add_job(src, win_s[j, :, 0:8 * wd], 8, wd)
            for dc in range(8):
                add_job(w_a[:, dc * 128:(dc + 1) * 128].rearrange("(c p) n -> p c n", p=128), wa_s[dc], 8, 128)
                add_job(w_b[:, dc * 128:(dc + 1) * 128].rearrange("(c p) n -> p c n", p=128), wb_s[dc], 8, 128)
            for dc in range(8):
                add_job(w_o[:, dc * 128:(dc + 1) * 128].rearrange("(c p) n -> p c n", p=128), wo_s[dc], 8, 128)
            for e in range(NE + 1):
                add_job(w_g[e].rearrange("(c p) n -> p c n", p=128), gu_s[e].rearrange("p (c n) -> p c n", c=8)[:, :, 0:256], 8, 256)
                add_job(w_u[e].rearrange("(c p) n -> p c n", p=128), gu_s[e].rearrange("p (c n) -> p c n", c=8)[:, :, 256:512], 8, 256)
                add_job(w_d[e].rearrange("(c p) n -> p c n", p=128), dn_s[e], 2, 1024)
            cast_eng = ["dve", "act", "pool"]

            def job_load(k):
                src_, dst, a, b = jobs[k]
                s = k % NS
                n = a * b
                s32v = stg32[s][:, 0:n].rearrange("p (a b) -> p a b", a=a)
                fw.dma("sp", sl[s], s32v, src_, writes=[T32[s]])

            def job_cast_store(k):
                src_, dst, a, b = jobs[k]
                s = k % NS
                n = a * b
                ce = cast_eng[k % 3]
                if ce == "act":
                    fw.op("act", I.copy(stg16[s][:, 0:n], stg32[s][:, 0:n]), [T32[s]], [T16[s]])
                else:
                    fw.op(ce, I.tensor_copy(stg16[s][:, 0:n], stg32[s][:, 0:n]), [T32[s]], [T16[s]])
                if len(dst.shape) == 3:
                    s16v = stg16[s][:, 0:n].rearrange("p (a b) -> p a b", a=a)
                else:
                    s16v = stg16[s][:, 0:n]
                fw.dma("sp", ss[s], dst, s16v, reads=[T16[s]])

            nj = len(jobs)
            LA = NS - 1
            for k in range(-LA, nj):
                if 0 <= k + LA < nj:
                    job_load(k + LA)
                if k >= 0:
                    job_cast_store(k)
            fw.barrier()
            fw.emit()

        sb = lambda name, shape, dt: st.enter_context(nc.sbuf_tensor("sb_" + name, shape, dt))
        cst = Buf(sb("cst", [128, NCST], F32))
        prm = Buf(sb("prm", [128, NPRM], F32))
        wr32 = Buf(sb("wr32", [128, 8, NE], F32))
        identb = Buf(sb("identb", [128, 128], BF16))
        onesb = Buf(sb("onesb", [128, 128], BF16))
        lbc = Buf(sb("lbc", [128, 16], F32))
        lngb = Buf(sb("lngb", [128, 2 * D], F32))
        S32 = Buf(sb("S32", [128, 8, 128], F32), 8)
        Sb = Buf(sb("Sb", [128, 8, 128], BF16), 8)
        C32 = Buf(sb("C32", [128, 4, 256], F32), 4)
        Cb = Buf(sb("Cb", [128, 4, 256], BF16), 4)
        halo = Buf(sb("halo", [128, 8, 4], F32), 8)
        xt = [Buf(sb("xt%d" % i, [128, D], F32)) for i in range(1)]
        R = Buf(sb("R", [128, 4, D], F32), 4)
        ACC = Buf(sb("ACC", [128, 4, D], F32), 4)
        hT = Buf(sb("hT", [128, 8, TB], BF16), 8)
        hA = Buf(sb("hA", [128, 8, TB], BF16), 8)
        sa16 = [Buf(sb("sa16_%d" % i, [128, TB], BF16)) for i in range(2)]
        yaT = Buf(sb("yaT", [128, 8, TB], BF16), 8)
        ybT = Buf(sb("ybT", [128, 8, TB], BF16), 8)
        mgT = Buf(sb("mgT", [128, 8, TB], BF16), 8)
        big16 = sb("big16", [128, 8, TB], F32)
        BIGT = Buf(big16, 8)
        vtokb_t = big16.bitcast(BF16)
        NF, NBF = 9, 9
        tf = [Buf(sb("tf%d" % i, [128, TB], F32)) for i in range(NF)]
        tb = [Buf(sb("tb%d" % i, [128, TB], BF16)) for i in range(NBF)]
        vtok = Buf(sb("vtok", [64, 8, 128], BF16))
        ktok = Buf(sb("ktok", [64, 8, 128], BF16))
        ATall = Buf(sb("ATall", [64, TB], BF16))
        Sv = [Buf(sb("Sv%d" % i, [128, NCH, 128], BF16)) for i in range(1)]
        svc = {"n": 0}
        Nv = Buf(sb("Nv", [128, NCH, 128], BF16))
        nst = Buf(sb("nst", [128, 24], F32))
        N32c = Buf(sb("N32c", [128, 4], F32))
        xpad = [Buf(sb("xpad%d" % i, [128, TB + 4], F32)) for i in range(1)]
        g4 = {n: Buf(sb("g4_" + n, [4, TB], F32)) for n in ("ig", "f", "P", "rP", "a")}
        lnst = [Buf(sb("lnst%d" % i, [128, 16], F32)) for i in range(2)]
        lnst4 = [Buf(sb("lnst4_%d" % i, [128, 16], F32)) for i in range(4)]
        rt = {n: Buf(sb("rt_" + n, [128, 64], F32)) for n in ("s", "bi", "ms", "sel", "gwu")}
        rt8 = Buf(sb("rt8", [128, 8, 8], F32))
        rts = Buf(sb("rts", [128, 40], F32))
        rtd = Buf(sb("rtd", [128, 2], F32))
        gw = Buf(sb("gw", [128, 8, NE], F32), 8)
        GU = [Buf(sb("GU%d" % i, [128, 8, 512], BF16)) for i in range(2)]
        DN = [Buf(sb("DN%d" % i, [128, 2, 1024], BF16)) for i in range(2)]
        GT = [Buf(sb("GT%d" % i, [128, 2, TB], BF16), 2) for i in range(2)]
        WT = [Buf(sb("WT%d" % i, [128, 8, 128], BF16)) for i in range(3)]
        print("sbuf bytes remaining:", nc.sbuf_bytes_remaining)
        banks = [Buf(st.enter_context(nc.psum_tensor("bank%d" % i, [128, 512], F32))) for i in range(8)]
        free_banks = list(range(4))
        free_banks_m = list(range(4, 8))
        for b_ in banks:
            b_.T.excl = True

        def pget():
            return banks[free_banks.pop(0)]

        def pput(b):
            k_ = banks.index(b)
            (free_banks if k_ < 4 else free_banks_m).append(k_)

        def mget():
            return banks[free_banks_m.pop(0)]

        s_x = [fw.new_sem("sx%d" % i) for i in range(2)]
        s_y = [fw.new_sem("sy%d" % i) for i in range(4)]
        s_wt = [fw.new_sem("swt%d" % i) for i in range(4)]
        s_gu = [fw.new_sem("sgu%d" % i) for i in range(2)]
        s_dn = [fw.new_sem("sdn%d" % i) for i in range(2)]
        s_ln = fw.new_sem("sln")
        s_c = fw.new_sem("sc")
        s_c2 = fw.new_sem("sc2")
        s_c3 = fw.new_sem("sc3")
        s_tap = fw.new_sem("stap")

        fw.dma("sp", s_c, cst.t[:], cst_d, writes=[cst.T])
        fw.dma("sp", s_c2, prm.t[:], prm_d, writes=[prm.T])
        fw.dma("sp", s_c3, wr32.t[:], w_r.rearrange("(c p) n -> p c n", p=128), writes=[wr32.T])
        V(I.tensor_copy(identb.t[:], cst.t[:, C_ID:C_ID + 128]), [cst.T], [identb.T])
        V(I.tensor_copy(onesb.t[:], cst.t[:, C_ONES:C_ONES + 128]), [cst.T], [onesb.T])
        V(I.tensor_tensor(lbc.t[:, 0:8], prm.t[:, P_LB0:P_LB0 + 8], prm.t[:, P_LB1:P_LB1 + 8], ALU.subtract), [prm.T], [lbc.T])
        A(I.activation(lbc.t[:, 0:8], lbc.t[:, 0:8], AF.Sigmoid), [lbc.T], [lbc.T])
        V(I.tensor_scalar(lbc.t[:, 8:16], lbc.t[:, 0:8], -1.0, 1.0, ALU.mult, ALU.add), [lbc.T], [lbc.T])
        for bufz in (S32, C32, N32c, halo):
            G(I.memset(bufz.t[:], 0.0), [], bufz.Ts)
        for bufz in (Sb, Cb):
            G(I.memset(bufz.t[:], 0.0), [], bufz.Ts)

        wt_state = {"n": 0}

        def load_wt(src_ap, ncol=128):
            k = wt_state["n"] % 3
            wt_state["n"] += 1
            b = WT[k]
            fw.dma("sp", s_wt[k], b.t[:, :, 0:ncol], src_ap.rearrange("p (c n) -> p c n", c=8), writes=[b.T])
            return b

        def win_tile(j):
            if j == 56:
                return load_wt(win_s[j, :, 0:64], 8)
            return load_wt(win_s[j])

        def proj_fm(bank, wt, ncol=128, m0=0):
            MM([I.matmul(bank.t[0:ncol, :], wt.t[:, c, m0:m0 + ncol], hA.t[:, c, :],
                                        start=(c == 0), stop=(c == 7)) for c in range(8)],
               [wt.T] + hA.Ts, [bank.T])

        lncnt = {"n": 0}

        def layer_norm(xap, xT, oap=None, oT=None):
            if oap is None:
                oap, oT = xap, xT
            k = lncnt["n"] % 2
            lncnt["n"] += 1
            s = lnst[k]
            V(I.bn_stats(s.t[:, 0:6], xap[:, 0:512]), [xT], [s.T])
            V(I.bn_stats(s.t[:, 6:12], xap[:, 512:1024]), [xT], [s.T])
            V(I.bn_aggr(s.t[:, 12:14], s.t[:, 0:12]), [s.T], [s.T])
            A(I.activation(s.t[:, 14:15], s.t[:, 13:14], AF.Sqrt, bias=float(EPS)), [s.T], [s.T])
            V(I.reciprocal(s.t[:, 14:15], s.t[:, 14:15]), [s.T], [s.T])
            V(I.tensor_scalar(oap, xap, s.t[:, 12:13], s.t[:, 14:15], ALU.subtract, ALU.mult), [xT, s.T], [oT])
            V(I.tensor_tensor(oap, oap, lngb.t[:, 0:D], ALU.mult), [oT, lngb.T], [oT])
            G(I.tensor_tensor(oap, oap, lngb.t[:, D:2 * D], ALU.add), [oT, lngb.T], [oT])

        def transpose_to_fm(src, dst32, dstb, dstTs_b, dstTs_32):
            for i in range(4):
                for g in range(2):
                    bk = pget()
                    MM([I.transpose(bk.t[:, (c % 4) * 128:(c % 4 + 1) * 128],
                                                   src.t[:, i, c * 128:(c + 1) * 128], cst.t[:, C_ID:C_ID + 128])
                        for c in range(4 * g, 4 * g + 4)], [src.Ts[i], cst.T], [bk.T])
                    bv = bk.t[:, :].rearrange("p (c t) -> p c t", c=4)
                    if dstb is not None:
                        A(I.copy(dstb[:, 4 * g:4 * g + 4, i * 128:(i + 1) * 128], bv),
                          [bk.T], [dstTs_b[g * 4 + i]])
                    if dst32 is not None:
                        if g == 0:
                            V(I.tensor_copy(dst32[:, 4 * g:4 * g + 4, i * 128:(i + 1) * 128], bv),
                              [bk.T], [dstTs_32[g * 4 + i]])
                        else:
                            A(I.copy(dst32[:, 4 * g:4 * g + 4, i * 128:(i + 1) * 128], bv),
                              [bk.T], [dstTs_32[g * 4 + i]])
                    pput(bk)

        def bc_last(buf_t, nparts, nrep, col0=63):
            base = buf_t[0:nparts, 0:1]
            return bass.AP(base.tensor, base.offset + col0, [[base.ap[0][0], nparts], [CH, NCH], [0, nrep]])

        _b = cst.t[:, C_NH:C_NH + 1]
        nh512 = bass.AP(_b.tensor, _b.offset, [[_b.ap[0][0], 128], [0, TB]])

        _m = cst.t[0:64, C_MASK:C_MASK + 1]
        mask_bc = bass.AP(_m.tensor, _m.offset, [[_m.ap[0][0], 64], [0, NCH], [1, CH]])

        def sv32(ch):
            return tf[7 + ch // 4].t[:, (ch % 4) * 128:(ch % 4 + 1) * 128]

        def v3(ap2d, a):
            return ap2d.rearrange("p (a b) -> p a b", a=a)

        def tap(name, ap, Ts):
            if name in tap_out:
                fw.dma("sp", s_tap, tap_out[name], ap, reads=Ts)

        def gen_front(blk):
            first = (blk == 0)
            fw.dma("sp", s_ln, lngb.t[:], lnp_d[0], writes=[lngb.T])
            for i in range(4):
                xb_ = xt[0]
                r0 = blk * TB + i * 128
                fw.dma("sp", s_x[0], xb_.t[:], xin[r0:r0 + 128, :], writes=[xb_.T])
                A(I.copy(R.t[:, i, :], xb_.t[:]), [xb_.T], [R.Ts[i]])
                layer_norm(R.t[:, i, :], R.Ts[i])
                yield 2
            transpose_to_fm(R, None, hA.t, hA.Ts, None)
            yield 4
            if first:
                tap("h", R.t[:, 0, :], [R.Ts[0]])

            for hd in range(8):
                p = 0
                F = lambda k: tf[p * 7 + k]
                Bq = lambda k: tb[p * 6 + k]
                fgt, kk, Pt, rP, nt, rstd, t1 = [F(k) for k in range(7)]
                kt, qt, qh, sg, sq, _ = [Bq(k) for k in range(6)]
                wq = win_tile(hd)
                bq = pget(); proj_fm(bq, wq)
                yield 1
                wf = win_tile(8 + hd)
                bf_ = pget(); proj_fm(bf_, wf)
                yield 1
                wg_ = win_tile(24 + hd)
                bg = pget(); proj_fm(bg, wg_)
                yield 1
                wv = win_tile(16 + hd)
                bvf = pget(); proj_fm(bvf, wv)
                vfm = tb[6]
                A(I.copy(vfm.t[:], bvf.t[:]), [bvf.T], [vfm.T])
                pput(bvf)
                yield 2
                bvT = pget()
                bvT16 = bvT.t.bitcast(BF16)
                MM([I.transpose(bvT16[0:64, ch * 128:(ch + 1) * 128], vfm.t[:, ch * 64:(ch + 1) * 64], identb.t[:])
                    for ch in range(NCH)], [vfm.T, identb.T], [bvT.T])
                A(I.copy(vtok.t[:, :, :], v3(bvT16[0:64, 0:1024], 8)), [bvT.T], [vtok.T])
                pput(bvT)
                yield 2
                A(I.activation(fgt.t[:], bf_.t[:], AF.Sigmoid), [bf_.T], [fgt.T])
                pput(bf_)
                A(I.activation(sg.t[:], bg.t[:], AF.Silu), [bg.T], [sg.T])
                pput(bg)
                V(I.tensor_scalar(fgt.t[:], fgt.t[:], lbc.t[:, 8 + hd:9 + hd], lbc.t[:, hd:hd + 1], ALU.mult, ALU.add),
                  [fgt.T, lbc.T], [fgt.T])
                V(I.tensor_scalar(kk.t[:], fgt.t[:], -1.0, 1.0, ALU.mult, ALU.add), [fgt.T], [kk.T])
                yield 1
                for ch in range(NCH):
                    V(I.tensor_tensor_scan(Pt.t[:, ch * 64:(ch + 1) * 64], fgt.t[:, ch * 64:(ch + 1) * 64],
                                                            cst.t[:, C_ZERO:C_ZERO + 64], 1.0, ALU.mult, ALU.add),
                      [fgt.T, cst.T], [Pt.T])
                V(I.reciprocal(rP.t[:], Pt.t[:]), [Pt.T], [rP.T])
                yield 1
                V(I.tensor_tensor(kk.t[:], kk.t[:], rP.t[:], ALU.mult), [kk.T, rP.T], [kk.T])
                V(I.tensor_tensor(v3(kt.t[:], NCH), v3(kk.t[:], NCH), bc_last(Pt.t, 128, CH), ALU.mult), [kk.T, Pt.T], [kt.T])
                yield 1
                V(I.tensor_tensor(fgt.t[:], bq.t[:], Pt.t[:], ALU.mult), [bq.T, Pt.T], [fgt.T])
                pput(bq)
                A(I.copy(qh.t[:], fgt.t[:]), [fgt.T], [qh.T])
                V(I.tensor_tensor(v3(qt.t[:], NCH), v3(fgt.t[:], NCH), bc_last(rP.t, 128, CH), ALU.mult), [fgt.T, rP.T], [qt.T])
                yield 1
                yield 4
                bkT = pget()
                bkT16 = bkT.t.bitcast(BF16)
                MM([I.transpose(bkT16[0:64, ch * 128:(ch + 1) * 128], kt.t[:, ch * 64:(ch + 1) * 64], identb.t[:])
                    for ch in range(NCH)], [kt.T, identb.T], [bkT.T])
                A(I.copy(ktok.t[:, :, :], v3(bkT16[0:64, 0:1024], 8)), [bkT.T], [ktok.T])
                pput(bkT)
                yield 2
                bsc = pget()
                MM([I.matmul(bsc.t[0:64, ch * 64:(ch + 1) * 64], kt.t[:, ch * 64:(ch + 1) * 64], qt.t[:, ch * 64:(ch + 1) * 64],
                             start=True, stop=True) for ch in range(NCH)], [kt.T, qt.T], [bsc.T])
                yield 1
                V(I.tensor_tensor(v3(ATall.t[:, :], NCH), v3(bsc.t[0:64, :], NCH), mask_bc, ALU.mult), [bsc.T, cst.T], [ATall.T])
                pput(bsc)
                bds = [pget(), pget()]
                for hb_ in range(2):
                    MM([I.matmul(bds[hb_].t[:, (ch % 4) * 128:(ch % 4 + 1) * 128], ktok.t[:, ch, :], vtok.t[:, ch, :], start=True, stop=True)
                        for ch in range(hb_ * 4, hb_ * 4 + 4)], [ktok.T, vtok.T], [bds[hb_].T])
                sv = Sv[0]
                svc["n"] += 1
                for ch in range(NCH):
                    pl = Pt.t[:, ch * 64 + 63:ch * 64 + 64]
                    bsl = bds[ch // 4].t[:, (ch % 4) * 128:(ch % 4 + 1) * 128]
                    prev = S32.t[:, hd, :] if ch == 0 else sv32(ch - 1)
                    V(I.scalar_tensor_tensor(sv32(ch), prev, pl, bsl, ALU.mult, ALU.add),
                      [S32.Ts[hd], Pt.T, bds[ch // 4].T, tf[7].T, tf[8].T], [tf[7 + ch // 4].T])
                    yield 1
                pput(bds[0]); pput(bds[1])
                A(I.copy(sv.t[:, 0:4, :], v3(tf[7].t[:], 4)), [tf[7].T], [sv.T])
                A(I.copy(sv.t[:, 4:8, :], v3(tf[8].t[:], 4)), [tf[8].T], [sv.T])
                G(I.tensor_copy(S32.t[:, hd, :], sv32(NCH - 1)), [tf[8].T], [S32.Ts[hd]])
                yield 8
                bo = pget()
                fns = []
                for ch in range(NCH):
                    cs = slice(ch * 64, (ch + 1) * 64)
                    sprev = Sb.t[:, hd, :] if ch == 0 else sv.t[:, ch - 1, :]
                    fns.append(I.matmul(bo.t[:, cs], vtok.t[:, ch, :], ATall.t[:, cs], start=True, stop=False))
                    fns.append(I.matmul(bo.t[:, cs], sprev, qh.t[:, cs], start=False, stop=True))
                MM(fns, [vtok.T, ATall.T, Sb.Ts[hd], sv.T, qh.T], [bo.T])
                G(I.tensor_copy(Sb.t[:, hd, :], sv.t[:, NCH - 1, :]), [sv.T], [Sb.Ts[hd]])
                yield 2
                A(I.activation(sq.t[:], bo.t[:], AF.Square), [bo.T], [sq.T])
                A(I.copy(t1.t[:], bo.t[:]), [bo.T], [t1.T])
                pput(bo)
                yield 3
                bm = pget()
                MM([I.matmul(bm.t[:], onesb.t[:], sq.t[:], start=True, stop=True)], [onesb.T, sq.T], [bm.T])
                yield 2
                A(I.activation(nt.t[:], bm.t[:], AF.Sqrt, bias=float(EPS), scale=1.0 / 128.0), [bm.T], [nt.T])
                pput(bm)
                V(I.reciprocal(rstd.t[:], nt.t[:]), [nt.T], [rstd.T])
                V(I.tensor_tensor(t1.t[:], t1.t[:], rstd.t[:], ALU.mult), [t1.T, rstd.T], [t1.T])
                yield 1
                V(I.scalar_tensor_tensor(yaT.t[:, hd, :], t1.t[:], prm.t[:, P_HGG + hd:P_HGG + hd + 1], sg.t[:], ALU.mult, ALU.mult),
                  [t1.T, prm.T, sg.T], [yaT.Ts[hd]])

            wgt = win_tile(56)
            big_ = pget(); proj_fm(big_, wgt, 4, 0)
            bfg = pget(); proj_fm(bfg, wgt, 4, 4)
            A(I.activation(g4["ig"].t[:], big_.t[0:4, :], AF.Exp, bias=prm.t[0:4, P_IGB:P_IGB + 1]), [big_.T, prm.T], [g4["ig"].T])
            A(I.activation(g4["f"].t[:], bfg.t[0:4, :], AF.Sigmoid, bias=prm.t[0:4, P_FGB:P_FGB + 1]), [bfg.T, prm.T], [g4["f"].T])
            pput(big_); pput(bfg)
            yield 4
            for ch in range(NCH):
                V(I.tensor_tensor_scan(g4["P"].t[:, ch * 64:(ch + 1) * 64], g4["f"].t[:, ch * 64:(ch + 1) * 64],
                                                        cst.t[0:4, C_ZERO:C_ZERO + 64], 1.0, ALU.mult, ALU.add),
                  [g4["f"].T, cst.T], [g4["P"].T])
                yield 1
            V(I.reciprocal(g4["rP"].t[:], g4["P"].t[:]), [g4["P"].T], [g4["rP"].T])
            V(I.tensor_tensor(g4["a"].t[:], g4["ig"].t[:], g4["rP"].t[:], ALU.mult), [g4["ig"].T, g4["rP"].T], [g4["a"].T])
            yield 1
            for hd in range(4):
                abc, pbc, dens, wsc, h0, h1_, mean, m2, var = [tf[k] for k in range(9)]
                qb, kb, khat, so0, so1, hs0, hs1, hb0, hb1 = [tb[k] for k in range(9)]
                for src, dstb in ((g4["a"], abc), (g4["P"], pbc)):
                    bb = pget()
                    MM([I.matmul(bb.t[:], cst.t[0:4, C_SEL + hd * 128:C_SEL + (hd + 1) * 128], src.t[:],
                                                                  start=True, stop=True)], [cst.T, src.T], [bb.T])
                    A(I.copy(dstb.t[:], bb.t[:]), [bb.T], [dstb.T])
                    pput(bb)
                    yield 1
                for which, dst in ((0, qb), (1, kb)):
                    j = 32 + which * 4 + hd
                    cidx = which * 4 + hd
                    wq = win_tile(j)
                    bq = pget(); proj_fm(bq, wq)
                    xp = xpad[0]
                    A(I.copy(xp.t[:, 4:4 + TB], bq.t[:]), [bq.T], [xp.T])
                    pput(bq)
                    yield 1
                    V(I.tensor_copy(xp.t[:, 0:4], halo.t[:, cidx, :]), [halo.Ts[cidx]], [xp.T])
                    V(I.tensor_copy(halo.t[:, cidx, :], xp.t[:, TB:TB + 4]), [xp.T], [halo.Ts[cidx]])
                    yield 1
                    cv = tf[7 + which]
                    V(I.tensor_scalar(cv.t[:], xp.t[:, 1:1 + TB], prm.t[:, P_CW + cidx:P_CW + cidx + 1],
                                                                        prm.t[:, P_CB + cidx:P_CB + cidx + 1], ALU.mult, ALU.add),
                      [xp.T, prm.T], [cv.T])
                    for tp in range(1, 4):
                        V(I.scalar_tensor_tensor(cv.t[:], xp.t[:, 1 + tp:1 + tp + TB],
                                                                                          prm.t[:, P_CW + tp * 8 + cidx:P_CW + tp * 8 + cidx + 1],
                                                                                          cv.t[:], ALU.mult, ALU.add),
                          [xp.T, prm.T, cv.T], [cv.T])
                        yield 1
                    A(I.activation(dst.t[:], cv.t[:], AF.Silu), [cv.T], [dst.T])
                V(I.tensor_scalar(qb.t[:], qb.t[:], 128.0 ** -0.5, None, ALU.mult), [qb.T], [qb.T])
                V(I.tensor_tensor(khat.t[:], kb.t[:], abc.t[:], ALU.mult), [kb.T, abc.T], [khat.T])
                yield 1
                for vt, so in ((0, so0), (1, so1)):
                    wq = win_tile(48 + hd * 2 + vt)
                    bq = pget(); proj_fm(bq, wq)
                    A(I.activation(so.t[:], bq.t[:], AF.Sigmoid), [bq.T], [so.T])
                    pput(bq)
                    yield 1
                vtb = vtokb_t
                for vt in range(2):
                    wv = win_tile(40 + hd * 2 + vt)
                    bvf = pget(); proj_fm(bvf, wv)
                    vfm = hs0
                    A(I.copy(vfm.t[:], bvf.t[:]), [bvf.T], [vfm.T])
                    pput(bvf)
                    yield 2
                    bvT = pget()
                    bvT16 = bvT.t.bitcast(BF16)
                    MM([I.transpose(bvT16[0:64, ch * 128:(ch + 1) * 128], vfm.t[:, ch * 64:(ch + 1) * 64], identb.t[:])
                        for ch in range(NCH)], [vfm.T, identb.T], [bvT.T])
                    A(I.copy(vtb[0:64, :, vt * 128:(vt + 1) * 128], v3(bvT16[0:64, 0:1024], 8)), [bvT.T], BIGT.Ts)
                    pput(bvT)
                    yield 2
                yield 3
                bkT = pget()
                bkT16 = bkT.t.bitcast(BF16)
                MM([I.transpose(bkT16[0:64, ch * 128:(ch + 1) * 128], khat.t[:, ch * 64:(ch + 1) * 64], identb.t[:])
                    for ch in range(NCH)], [khat.T, identb.T], [bkT.T])
                V(I.tensor_tensor(ktok.t[:, :, :], v3(bkT16[0:64, 0:1024], 8), bc_last(pbc.t, 64, 128), ALU.mult),
                  [bkT.T, pbc.T], [ktok.T])
                pput(bkT)
                yield 2
                bsc = pget()
                MM([I.matmul(bsc.t[0:64, ch * 64:(ch + 1) * 64], khat.t[:, ch * 64:(ch + 1) * 64], qb.t[:, ch * 64:(ch + 1) * 64],
                             start=True, stop=True) for ch in range(NCH)], [khat.T, qb.T], [bsc.T])
                yield 1
                V(I.tensor_tensor(v3(ATall.t[:, :], NCH), v3(bsc.t[0:64, :], NCH), mask_bc, ALU.mult), [bsc.T, cst.T], [ATall.T])
                yield 1
                pput(bsc)
                V(I.tensor_reduce(nst.t[:, 16:24], v3(khat.t[:], NCH), mybir.AxisListType.X, ALU.add), [khat.T], [nst.T])
                V(I.tensor_copy(nst.t[:, 0:1], N32c.t[:, hd:hd + 1]), [N32c.T], [nst.T])
                yield 1
                for ch in range(NCH):
                    V(I.scalar_tensor_tensor(nst.t[:, ch + 1:ch + 2], nst.t[:, ch:ch + 1], nst.t[:, 16 + ch:17 + ch],
                                             pbc.t[:, ch * 64 + 63:ch * 64 + 64], ALU.add, ALU.mult), [nst.T, pbc.T], [nst.T])
                V(I.tensor_copy(N32c.t[:, hd:hd + 1], nst.t[:, 8:9]), [nst.T], [N32c.T])
                yield 1
                _nb = nst.t[:, 0:1]
                V(I.tensor_copy(Nv.t[:, :, :], bass.AP(_nb.tensor, _nb.offset, [[_nb.ap[0][0], 128], [1, NCH], [0, 128]])), [nst.T], [Nv.T])
                yield 3
                bdn = pget()
                fns = []
                for ch in range(NCH):
                    cs = slice(ch * 64, (ch + 1) * 64)
                    fns.append(I.matmul(bdn.t[:, cs], onesb.t[0:64, :], ATall.t[:, cs], start=True, stop=False))
                    fns.append(I.matmul(bdn.t[:, cs], Nv.t[:, ch, :], qb.t[:, cs], start=False, stop=True))
                MM(fns, [onesb.T, ATall.T, Nv.T, qb.T], [bdn.T])
                V(I.tensor_tensor(dens.t[:], bdn.t[:], pbc.t[:], ALU.mult), [bdn.T, pbc.T], [dens.T])
                yield 1
                pput(bdn)
                bns = []
                for vt in range(2):
                    vsl = slice(vt * 128, (vt + 1) * 128)
                    bds = [pget(), pget()]
                    for hb_ in range(2):
                        MM([I.matmul(bds[hb_].t[:, (ch % 4) * 128:(ch % 4 + 1) * 128], ktok.t[:, ch, :], vtb[0:64, ch, vsl], start=True, stop=True)
                            for ch in range(hb_ * 4, hb_ * 4 + 4)], [ktok.T] + BIGT.Ts, [bds[hb_].T])
                    sv = Sv[0]
                    svc["n"] += 1
                    for ch in range(NCH):
                        pl = pbc.t[:, ch * 64 + 63:ch * 64 + 64]
                        bsl = bds[ch // 4].t[:, (ch % 4) * 128:(ch % 4 + 1) * 128]
                        prev = C32.t[:, hd, vsl] if ch == 0 else sv32(ch - 1)
                        V(I.scalar_tensor_tensor(sv32(ch), prev, pl, bsl, ALU.mult, ALU.add),
                          [C32.Ts[hd], pbc.T, bds[ch // 4].T, tf[7].T, tf[8].T], [tf[7 + ch // 4].T])
                    pput(bds[0]); pput(bds[1])
                    A(I.copy(sv.t[:, 0:4, :], v3(tf[7].t[:], 4)), [tf[7].T], [sv.T])
                    A(I.copy(sv.t[:, 4:8, :], v3(tf[8].t[:], 4)), [tf[8].T], [sv.T])
                    G(I.tensor_copy(C32.t[:, hd, vsl], sv32(NCH - 1)), [tf[8].T], [C32.Ts[hd]])
                    yield 8
                    bn = pget()
                    fns = []
                    for ch in range(NCH):
                        cs = slice(ch * 64, (ch + 1) * 64)
                        cprev = Cb.t[:, hd, vsl] if ch == 0 else sv.t[:, ch - 1, :]
                        fns.append(I.matmul(bn.t[:, cs], vtb[0:64, ch, vsl], ATall.t[:, cs], start=True, stop=False))
                        fns.append(I.matmul(bn.t[:, cs], cprev, qb.t[:, cs], start=False, stop=True))
                    MM(fns, BIGT.Ts + [ATall.T, Cb.Ts[hd], sv.T, qb.T], [bn.T])
                    G(I.tensor_copy(Cb.t[:, hd, vsl], sv.t[:, NCH - 1, :]), [sv.T], [Cb.Ts[hd]])
                    hh_ = (h0, h1_)[vt]
                    A(I.copy(hh_.t[:], bn.t[:]), [bn.T], [hh_.T])
                    pput(bn)
                    yield 2
                A(I.activation(dens.t[:], dens.t[:], AF.Abs), [dens.T], [dens.T])
                V(I.tensor_scalar(dens.t[:], dens.t[:], 1.0, None, ALU.max), [dens.T], [dens.T])
                yield 1
                V(I.reciprocal(dens.t[:], dens.t[:]), [dens.T], [dens.T])
                V(I.tensor_tensor(wsc.t[:], pbc.t[:], dens.t[:], ALU.mult), [pbc.T, dens.T], [wsc.T])
                yield 1
                for hh, hs, hb in ((h0, hs0, hb0), (h1_, hs1, hb1)):
                    V(I.tensor_tensor(hh.t[:], hh.t[:], wsc.t[:], ALU.mult), [hh.T, wsc.T], [hh.T])
                    A(I.activation(hs.t[:], hh.t[:], AF.Square), [hh.T], [hs.T])
                    A(I.copy(hb.t[:], hh.t[:]), [hh.T], [hb.T])
                yield 8
                bsu = pget(); bsq = pget()
                MM([I.matmul(bsu.t[:], onesb.t[:], hb0.t[:], start=True, stop=False),
                    I.matmul(bsu.t[:], onesb.t[:], hb1.t[:], start=False, stop=True)], [onesb.T, hb0.T, hb1.T], [bsu.T])
                MM([I.matmul(bsq.t[:], onesb.t[:], hs0.t[:], start=True, stop=False),
                    I.matmul(bsq.t[:], onesb.t[:], hs1.t[:], start=False, stop=True)], [onesb.T, hs0.T, hs1.T], [bsq.T])
                A(I.activation(mean.t[:], bsu.t[:], AF.Identity, scale=1.0 / 256.0), [bsu.T], [mean.T])
                pput(bsu)
                V(I.tensor_tensor(m2.t[:], mean.t[:], mean.t[:], ALU.mult), [mean.T], [m2.T])
                yield 1
                V(I.scalar_tensor_tensor(var.t[:], bsq.t[:], 1.0 / 256.0, m2.t[:], ALU.mult, ALU.subtract), [bsq.T, m2.T], [var.T])
                pput(bsq)
                A(I.activation(var.t[:], var.t[:], AF.Sqrt, bias=float(EPS)), [var.T], [var.T])
                V(I.reciprocal(var.t[:], var.t[:]), [var.T], [var.T])
                yield 1
                for vt, hh, so in ((0, h0, so0), (1, h1_, so1)):
                    V(I.tensor_tensor(hh.t[:], hh.t[:], mean.t[:], ALU.subtract), [hh.T, mean.T], [hh.T])
                    V(I.tensor_tensor(hh.t[:], hh.t[:], var.t[:], ALU.mult), [hh.T, var.T], [hh.T])
                    yield 1
                    cg = P_MLG + hd * 2 + vt
                    V(I.scalar_tensor_tensor(ybT.t[:, hd * 2 + vt, :], hh.t[:], prm.t[:, cg:cg + 1], so.t[:], ALU.mult, ALU.mult),
                      [hh.T, prm.T, so.T], [ybT.Ts[hd * 2 + vt]])

            for dc in range(8):
                wa = load_wt(wa_s[dc])
                ba = pget()
                MM([I.matmul(ba.t[:], wa.t[:, c, :], yaT.t[:, c, :], start=(c == 0), stop=(c == 7)) for c in range(8)],
                   [wa.T] + yaT.Ts, [ba.T])
                wbt = load_wt(wb_s[dc])
                bb = pget()
                MM([I.matmul(bb.t[:], wbt.t[:, c, :], ybT.t[:, c, :], start=(c == 0), stop=(c == 7)) for c in range(8)],
                   [wbt.T] + ybT.Ts, [bb.T])
                wga = win_tile(57 + dc)
                yield 2
                bga = pget(); proj_fm(bga, wga)
                wgb = win_tile(65 + dc)
                bgb = pget(); proj_fm(bgb, wgb)
                sga, sgb, m1 = tf[0], tf[1], tf[2]
                A(I.activation(sga.t[:], bga.t[:], AF.Sigmoid), [bga.T], [sga.T])
                A(I.activation(sgb.t[:], bgb.t[:], AF.Sigmoid), [bgb.T], [sgb.T])
                pput(bga); pput(bgb)
                V(I.tensor_tensor(m1.t[:], ba.t[:], sga.t[:], ALU.mult), [ba.T, sga.T], [m1.T])
                yield 1
                V(I.tensor_tensor(sgb.t[:], bb.t[:], sgb.t[:], ALU.mult), [bb.T, sgb.T], [sgb.T])
                pput(ba); pput(bb)
                V(I.tensor_tensor(mgT.t[:, dc, :], m1.t[:], sgb.t[:], ALU.add), [m1.T, sgb.T], [mgT.Ts[dc]])
                yield 1
                yield 2

            fw.dma("sp", s_ln, lngb.t[:], lnp_d[1], writes=[lngb.T])
            for hf in range(2):
                bks = [pget() for _ in range(4)]
                for dcl in range(4):
                    wt = load_wt(wo_s[hf * 4 + dcl])
                    for i in range(4):
                        MM([I.matmul(bks[i].t[:, dcl * 128:(dcl + 1) * 128], mgT.t[:, c, i * 128:(i + 1) * 128], wt.t[:, c, :],
                                     start=(c == 0), stop=(c == 7)) for c in range(8)], mgT.Ts + [wt.T], [bks[i].T])
                    yield 2
                for i in range(4):
                    V(I.scalar_tensor_tensor(R.t[:, i, hf * 512:(hf + 1) * 512], R.t[:, i, hf * 512:(hf + 1) * 512],
                                             float(ALPHA), bks[i].t[:], ALU.mult, ALU.add), [R.Ts[i], bks[i].T], [R.Ts[i]])
                    pput(bks[i])
                yield 1
            for i in range(4):
                layer_norm(R.t[:, i, :], R.Ts[i])
                yield 1
            if first:
                tap("h1", R.t[:, 0, :], [R.Ts[0]])

            transpose_to_fm(R, big16, None, None, BIGT.Ts)
            yield 3
            par = blk % 2
            for i in range(4):
                bk = pget()
                MM([I.matmul(bk.t[:, 0:NE], big16[:, c, i * 128:(i + 1) * 128], wr32.t[:, c, :],
                                                       start=(c == 0), stop=(c == 7)) for c in range(8)],
                   BIGT.Ts + [wr32.T], [bk.T])
                s_, bi, ms_, sel, gwu = rt["s"], rt["bi"], rt["ms"], rt["sel"], rt["gwu"]
                A(I.activation(s_.t[:], bk.t[:, 0:NE], AF.Sigmoid), [bk.T], [s_.T])
                pput(bk)
                V(I.tensor_tensor(bi.t[:], s_.t[:], cst.t[:, C_RB:C_RB + NE], ALU.add), [s_.T, cst.T], [bi.T])
                yield 1
                for g in range(8):
                    V(I.max(rt8.t[:, g, :], bi.t[:, g * 8:(g + 1) * 8]), [bi.T], [rt8.T])
                V(I.tensor_tensor(rts.t[:, 0:8], rt8.t[:, :, 0], rt8.t[:, :, 1], ALU.add), [rt8.T], [rts.T])
                yield 1
                V(I.max(rts.t[:, 8:16], rts.t[:, 0:8]), [rts.T], [rts.T])
                V(I.tensor_scalar(rts.t[:, 16:24], rts.t[:, 0:8], rts.t[:, 11:12], None, ALU.is_ge), [rts.T], [rts.T])
                yield 1
                V(I.tensor_scalar(rts.t[:, 24:32], rts.t[:, 16:24], BIG, -BIG, ALU.mult, ALU.add), [rts.T], [rts.T])


                def gb8(c0):
                    base = rts.t[:, c0:c0 + 1]
                    return bass.AP(base.tensor, base.offset, [[base.ap[0][0], 128], [1, 8], [0, 8]])
                V(I.tensor_tensor(v3(ms_.t[:], 8), v3(bi.t[:], 8), gb8(16), ALU.mult), [bi.T, rts.T], [ms_.T])
                yield 1
                V(I.tensor_tensor(v3(ms_.t[:], 8), v3(ms_.t[:], 8), gb8(24), ALU.add), [ms_.T, rts.T], [ms_.T])
                V(I.max(rts.t[:, 32:40], ms_.t[:]), [ms_.T], [rts.T])
                yield 1
                V(I.tensor_scalar(sel.t[:], ms_.t[:], rts.t[:, 39:40], None, ALU.is_ge), [ms_.T, rts.T], [sel.T])
                V(I.tensor_tensor(gwu.t[:], s_.t[:], sel.t[:], ALU.mult), [s_.T, sel.T], [gwu.T])
                yield 1
                V(I.tensor_reduce(rtd.t[:, 0:1], gwu.t[:], mybir.AxisListType.X, ALU.add), [gwu.T], [rtd.T])
                V(I.reciprocal(rtd.t[:, 1:2], rtd.t[:, 0:1]), [rtd.T], [rtd.T])
                yield 1
                V(I.tensor_scalar(gw.t[:, par * 4 + i, :], gwu.t[:], rtd.t[:, 1:2], 2.5, ALU.mult, ALU.mult), [gwu.T, rtd.T], [gw.Ts[par * 4 + i]])
                yield 2
            if first:
                tap("gw", gw.t[:, 0, :], [gw.Ts[0]])

        def gen_moe(blk):
            par = blk % 2

            def load_expert(e):
                k = e % 2
                fw.dma("sp", s_gu[k], GU[k].t[:], gu_s[e].rearrange("p (c n) -> p c n", c=8), writes=[GU[k].T])
                fw.dma("sp", s_dn[k], DN[k].t[:], dn_s[e].rearrange("p (c n) -> p c n", c=2), writes=[DN[k].T])
            load_expert(0)
            for e in range(NE + 1):
                if e + 1 <= NE:
                    load_expert(e + 1)
                k = e % 2
                gu, dn, gt = GU[k], DN[k], GT[k]
                for ft in range(2):
                    bga = mget()
                    MM([I.matmul(bga.t[:], gu.t[:, c, ft * 128:(ft + 1) * 128], hT.t[:, c, :],
                                                                      start=(c == 0), stop=(c == 7)) for c in range(8)],
                       [gu.T] + hT.Ts, [bga.T])
                    bup = mget()
                    MM([I.matmul(bup.t[:], gu.t[:, c, 256 + ft * 128:256 + (ft + 1) * 128], hT.t[:, c, :],
                                                                      start=(c == 0), stop=(c == 7)) for c in range(8)],
                       [gu.T] + hT.Ts, [bup.T])
                    sa = sa16[ft]
                    A(I.activation(sa.t[:], bga.t[:], AF.Silu), [bga.T], [sa.T])
                    pput(bga)
                    V(I.tensor_tensor(gt.t[:, ft, :], bup.t[:], sa.t[:], ALU.mult), [bup.T, sa.T], [gt.Ts[ft]])
                    pput(bup)
                    yield 1
                for i in range(4):
                    for hf in range(2):
                        bk = mget()
                        MM([I.matmul(bk.t[:], gt.t[:, kk_, i * 128:(i + 1) * 128],
                                                                                       dn.t[:, kk_, hf * 512:(hf + 1) * 512],
                                                                                       start=(kk_ == 0), stop=(kk_ == 1)) for kk_ in range(2)],
                           gt.Ts + [dn.T], [bk.T])
                        acc = ACC.t[:, i, hf * 512:(hf + 1) * 512]
                        if e < NE:
                            V(I.scalar_tensor_tensor(acc, bk.t[:], gw.t[:, par * 4 + i, e:e + 1], acc, ALU.mult, ALU.add),
                              [bk.T, gw.Ts[par * 4 + i], ACC.Ts[i]], [ACC.Ts[i]])
                        else:
                            V(I.tensor_tensor(acc, bk.t[:], acc, ALU.add), [bk.T, ACC.Ts[i]], [ACC.Ts[i]])
                        pput(bk)
                        yield 1

        def moe_cast():
            for c in range(8):
                if c % 2 == 0:
                    A(I.copy(hT.t[:, c, :], big16[:, c, :]), BIGT.Ts, [hT.Ts[c]])
                else:
                    V(I.tensor_copy(hT.t[:, c, :], big16[:, c, :]), BIGT.Ts, [hT.Ts[c]])

        def acc_init(i):
            A(I.activation(ACC.t[:, i, :], R.t[:, i, :], AF.Copy, scale=float(ALPHA)), [R.Ts[i]], [ACC.Ts[i]])

        def stage_h(blk):
            fw.dma("sp", s_ln, lngb.t[:], lnp_d[2], writes=[lngb.T])
            for i in range(4):
                s = lnst4[i]
                xap = ACC.t[:, i, :]
                V(I.bn_stats(s.t[:, 0:6], xap[:, 0:512]), [ACC.Ts[i]], [s.T])
                V(I.bn_stats(s.t[:, 6:12], xap[:, 512:1024]), [ACC.Ts[i]], [s.T])
                V(I.bn_aggr(s.t[:, 12:14], s.t[:, 0:12]), [s.T], [s.T])
                A(I.activation(s.t[:, 14:15], s.t[:, 13:14], AF.Sqrt, bias=float(EPS)), [s.T], [s.T])
                V(I.reciprocal(s.t[:, 14:15], s.t[:, 14:15]), [s.T], [s.T])
            stg = [(xt[0].t[:], xt[0].T), (R.t[:, 0, :], R.Ts[0]), (R.t[:, 1, :], R.Ts[1]), (R.t[:, 2, :], R.Ts[2])]
            for i in range(4):
                s = lnst4[i]
                oap, oT = stg[i]
                V(I.tensor_scalar(oap, ACC.t[:, i, :], s.t[:, 12:13], s.t[:, 14:15], ALU.subtract, ALU.mult), [ACC.Ts[i], s.T], [oT])
                if blk + 1 < NB:
                    acc_init(i)
                V(I.tensor_tensor(oap, oap, lngb.t[:, 0:D], ALU.mult), [oT, lngb.T], [oT])
                G(I.tensor_tensor(oap, oap, lngb.t[:, D:2 * D], ALU.add), [oT, lngb.T], [oT])
                r0 = blk * TB + i * 128
                fw.dma("pool", s_y[i], y[r0:r0 + 128, :], oap, reads=[oT])

        def run_all(g):
            for _ in g:
                pass

        def interleave(gm, gf, per):
            acc = 0.0
            m_alive = True
            for _ in range(HEAD_START):
                next(gm)
            wtot = [0]
            for w in gf:
                wtot[0] += w
                acc += w * per
                while m_alive and acc >= 1.0:
                    try:
                        next(gm)
                    except StopIteration:
                        m_alive = False
                    acc -= 1.0
            left = 0
            if m_alive:
                for _ in gm:
                    left += 1
            if not hasattr(interleave, "_p"):
                interleave._p = True
                print("interleave: front weight total", wtot[0], "moe units left after front", left)

        run_all(gen_front(0))
        moe_cast()
        for i in range(4):
            acc_init(i)
        for blk in range(NB):
            gm = gen_moe(blk)
            if blk + 1 < NB:
                interleave(gm, gen_front(blk + 1), RATIO)
                moe_cast()
            else:
                run_all(gm)
            stage_h(blk)
        fw.barrier()
        fw.emit()
        print("ops:", fw.n_ops)
    return nc


def host_consts(router_bias):
    c = np.zeros((128, NCST), np.float32)
    c[:, C_ID:C_ID + 128] = np.eye(128, dtype=np.float32)
    p = np.arange(128)[:, None] % 64
    t = np.arange(64)[None, :]
    c[:, C_MASK:C_MASK + 64] = (p <= t).astype(np.float32)
    c[:, C_ONES:C_ONES + 128] = 1.0
    c[:, C_NH:C_NH + 1] = -0.5
    for h in range(4):
        c[h, C_SEL + h * 128:C_SEL + (h + 1) * 128] = 1.0
    c[:, C_RB:C_RB + NE] = np.asarray(router_bias, np.float32).reshape(1, NE)
    return c


def fm(v):
    v = np.asarray(v, np.float32)
    return np.ascontiguousarray(v.reshape(-1, 128).T)


def host_params(inp):
    p = np.zeros((128, NPRM), np.float32)
    p[:, P_LB0:P_LB0 + 8] = fm(inp["hg_lb_logits"][0])
    p[:, P_LB1:P_LB1 + 8] = fm(inp["hg_lb_logits"][1])
    p[:, P_HGG:P_HGG + 8] = fm(inp["hg_norm_g"][0])
    for tp in range(4):
        p[:, P_CW + tp * 8:P_CW + tp * 8 + 8] = fm(inp["ml_conv_w"][0, tp])
    p[:, P_CB:P_CB + 8] = fm(inp["ml_conv_b"][0])
    p[:, P_MLG:P_MLG + 8] = fm(inp["ml_norm_g"][0])
    p[0:4, P_IGB] = np.asarray(inp["ml_ig_bias"][0], np.float32)
    p[0:4, P_FGB] = np.asarray(inp["ml_fg_bias"][0], np.float32)
    return p


def host_shared(inp):
    f = lambda a: np.ascontiguousarray(np.asarray(a, np.float32))
    lnp = np.zeros((3, 128, 2 * D), np.float32)
    for k, (g, b) in enumerate((("ln_emb_g", "ln_emb_b"), ("ln1_g", "ln1_b"), ("ln2_g", "ln2_b"))):
        lnp[k, :, 0:D] = np.asarray(inp[g], np.float32).reshape(1, D)
        lnp[k, :, D:] = np.asarray(inp[b], np.float32).reshape(1, D)
    return {
        "w_in": f(inp["w_in"][0]), "w_a": f(inp["w_branch_a"][0]), "w_b": f(inp["w_branch_b"][0]),
        "w_o": f(inp["w_out"][0]), "w_r": f(inp["w_router"][0]),
        "w_g": np.concatenate([f(inp["w_exp_gate"][0]), f(inp["w_sh_gate"][0])[None]], 0),
        "w_u": np.concatenate([f(inp["w_exp_up"][0]), f(inp["w_sh_up"][0])[None]], 0),
        "w_d": np.concatenate([f(inp["w_exp_down"][0]), f(inp["w_sh_down"][0])[None]], 0),
        "cst": host_consts(inp["router_bias"][0]), "prm": host_params(inp), "lnp": lnp,
    }


def make_xin(x_b, meta, NB):
    xin = np.zeros((NB * TB, D), np.float32)
    xin[0:NMETA] = meta
    n = x_b.shape[0]
    xin[NMETA:NMETA + n] = x_b
    return xin


_NC_CACHE = {}


def kernel(**inputs):
    x = np.asarray(inputs["x"], np.float32)
    B, S, _ = x.shape
    NB = -(-(S + NMETA) // TB)
    shared = host_shared(inputs)
    meta = np.asarray(inputs["meta_tokens"], np.float32)
    if NB not in _NC_CACHE:
        _NC_CACHE[NB] = build(NB)
    nc = _NC_CACHE[NB]
    in_maps = []
    for b in range(B):
        m = dict(shared)
        m["xin"] = make_xin(x[b], meta, NB)
        in_maps.append(m)
    res = run_bass_kernel_spmd(nc, in_maps, core_ids=list(range(B)))
    out = np.stack([r["y"][NMETA:NMETA + S] for r in res.results], 0)
    return out.astype(np.float32)
```

```python
import numpy as np
from contextlib import ExitStack
import ml_dtypes
import concourse.bass as bass
import concourse.mybir as mybir
from concourse.bass_utils import run_bass_kernel_spmd

F32 = mybir.dt.float32
BF16 = mybir.dt.bfloat16
AF = mybir.ActivationFunctionType
ALU = mybir.AluOpType

D = 1024
SEQ = 8192
NMETA = 16
TB = 512
CH = 64
NCH = TB // CH
NBLK = 17
EPS = 1e-5
ALPHA = 2.0 ** 0.25
IN_W = 9224
NE = 64
BIG = 1.0e30
import os
RATIO = float(os.environ.get("KRATIO", "0.8"))
HEAD_START = int(os.environ.get("KHEAD", "40"))

C_ID = 0
C_MASK = 128
C_ONES = 192
C_NH = 320
C_SEL = 321
C_RB = 833
C_ZERO = 897
NCST = 961
P_LB0, P_LB1, P_HGG, P_CW, P_CB, P_MLG, P_IGB, P_FGB = 0, 8, 16, 24, 56, 64, 72, 73
NPRM = 74


class Sem:
    def __init__(self, h, name):
        self.h = h
        self.name = name
        self.val = 0


class T:
    __slots__ = ("name", "w", "r", "excl")

    def __init__(self, name=""):
        self.name = name
        self.w = None
        self.r = {}
        self.excl = False


class Eng:
    def __init__(self, name, sem):
        self.name = name
        self.sem = sem
        self.seen = {}
        self.ops = []


class FW:
    def __init__(self, nc, stack):
        self.nc = nc
        self.stack = stack
        self.sems = []
        self.engs = {}
        for n in ("pe", "act", "dve", "pool", "sp"):
            self.engs[n] = Eng(n, self.new_sem("ev_" + n))
        self.n_ops = 0

    def new_sem(self, name):
        h = self.stack.enter_context(self.nc.semaphore(name))
        s = Sem(h, name)
        self.sems.append(s)
        return s

    def _collect(self, e, reads, writes):
        need = {}
        if any(t.excl for t in reads):
            writes = list(writes) + [t for t in reads if t.excl]
            reads = [t for t in reads if not t.excl]

        def add(ev, same_ok):
            if ev is None:
                return
            s, v = ev
            if same_ok and s is e.sem and e.name == "pe":
                return
            if e.seen.get(s, 0) >= v:
                return
            if need.get(s, 0) < v:
                need[s] = v

        for t in reads:
            add(t.w, False)
        for t in writes:
            add(t.w, True)
            for s, v in t.r.items():
                add((s, v), True)
        for s, v in need.items():
            e.seen[s] = v
        return [(s.h, v) for s, v in need.items()]

    def _commit(self, ev, reads, writes):
        s, v = ev
        if any(t.excl for t in reads):
            writes = list(writes) + [t for t in reads if t.excl]
            reads = [t for t in reads if not t.excl]
        for t in reads:
            if t.r.get(s, 0) < v:
                t.r[s] = v
        for t in writes:
            t.w = ev
            t.r = {}

    def op(self, eng, fn, reads=(), writes=()):
        e = self.engs[eng]
        waits = self._collect(e, reads, writes)
        e.sem.val += 1
        ev = (e.sem, e.sem.val)
        semh = e.sem.h

        m, a, kw = fn

        def run(h):
            for sh, v in waits:
                h.wait_ge(sh, v)
            getattr(h, m)(*a, **kw).then_inc(semh, 1)
        e.ops.append(run)
        self._commit(ev, reads, writes)
        self.n_ops += 1

    def group(self, eng, fns, reads=(), writes=()):
        e = self.engs[eng]
        waits = self._collect(e, reads, writes)
        e.sem.val += 1
        ev = (e.sem, e.sem.val)
        semh = e.sem.h

        def run(h):
            for sh, v in waits:
                h.wait_ge(sh, v)
            for (m, a, kw) in fns[:-1]:
                getattr(h, m)(*a, **kw)
            m, a, kw = fns[-1]
            getattr(h, m)(*a, **kw).then_inc(semh, 1)
        e.ops.append(run)
        self._commit(ev, reads, writes)
        self.n_ops += len(fns)

    def dma(self, q, sem, out, in_, reads=(), writes=()):
        e = self.engs[q]
        waits = self._collect(e, reads, writes)
        sem.val += 16
        ev = (sem, sem.val)
        semh = sem.h

        def run(h):
            for sh, v in waits:
                h.wait_ge(sh, v)
            h.dma_start(out=out, in_=in_).then_inc(semh, 16)
        e.ops.append(run)
        self._commit(ev, reads, writes)
        self.n_ops += 1

    def barrier(self):
        for e in self.engs.values():
            lst = []
            for s in self.sems:
                if s.val > 0 and e.seen.get(s, 0) < s.val:
                    lst.append((s.h, s.val))
                    e.seen[s] = s.val

            def run(h, lst=lst):
                for sh, v in lst:
                    h.wait_ge(sh, v)
            e.ops.append(run)

    def emit(self):
        nc = self.nc
        engs = self.engs
        with nc.Block() as block:
            @block.sync
            def _(h):
                for f in engs["sp"].ops:
                    f(h)

            @block.tensor
            def _(h):
                for f in engs["pe"].ops:
                    f(h)

            @block.scalar
            def _(h):
                for f in engs["act"].ops:
                    f(h)

            @block.vector
            def _(h):
                for f in engs["dve"].ops:
                    f(h)

            @block.gpsimd
            def _(h):
                for f in engs["pool"].ops:
                    f(h)
        for e in engs.values():
            e.ops = []


class Buf:
    def __init__(self, t, nT=1, name=""):
        self.t = t
        self.Ts = [T(name + str(i)) for i in range(nT)]

    @property
    def T(self):
        return self.Ts[0]


def w_in_col0(j):
    if j < 56:
        return 128 * j
    if j == 56:
        return 7168
    if j < 65:
        return 7176 + 128 * (j - 57)
    return 8200 + 128 * (j - 65)


class _Stop(Exception):
    pass


def build(NB=NBLK, taps=(), stop=None):
    nc = bass.Bass("TRN2", target_bir_lowering=False)
    NT = NB * TB
    dram = lambda n, shp, dt=F32, kind="ExternalInput": nc.dram_tensor(n, shp, dt, kind=kind).ap()
    xin = dram("xin", [NT, D])
    w_in = dram("w_in", [D, IN_W])
    w_a = dram("w_a", [D, D])
    w_b = dram("w_b", [D, D])
    w_o = dram("w_o", [D, D])
    w_r = dram("w_r", [D, NE])
    w_g = dram("w_g", [NE + 1, D, 256])
    w_u = dram("w_u", [NE + 1, D, 256])
    w_d = dram("w_d", [NE + 1, 256, D])
    cst_d = dram("cst", [128, NCST])
    prm_d = dram("prm", [128, NPRM])
    lnp_d = dram("lnp", [3, 128, 2 * D])
    y = dram("y", [NT, D], F32, "ExternalOutput")
    win_s = dram("win_s", [73, 128, 8 * 128], BF16, "Internal")
    wa_s = dram("wa_s", [8, 128, 8 * 128], BF16, "Internal")
    wb_s = dram("wb_s", [8, 128, 8 * 128], BF16, "Internal")
    wo_s = dram("wo_s", [8, 128, 8 * 128], BF16, "Internal")
    gu_s = dram("gu_s", [NE + 1, 128, 8 * 512], BF16, "Internal")
    dn_s = dram("dn_s", [NE + 1, 128, 2 * 1024], BF16, "Internal")
    tap_out = {}
    for name, shp in taps:
        tap_out[name] = dram("tap_" + name, list(shp), F32, "ExternalOutput")

    with ExitStack() as st:
        fw = FW(nc, st)
        V = lambda fn, r=(), w=(): fw.op("dve", fn, r, w)
        A = lambda fn, r=(), w=(): fw.op("act", fn, r, w)
        G = lambda fn, r=(), w=(): fw.op("pool", fn, r, w)
        MM = lambda fns, r=(), w=(): fw.group("pe", fns, r, w)

        class _Rec:
            def __getattr__(self, m):
                return lambda *a, **kw: (m, a, kw)
        I = _Rec()

        with ExitStack() as pst:
            NS = 8
            stg32 = [pst.enter_context(nc.sbuf_tensor("stg32_%d" % i, [128, 4096], F32)) for i in range(NS)]
            stg16 = [pst.enter_context(nc.sbuf_tensor("stg16_%d" % i, [128, 4096], BF16)) for i in range(NS)]
            T32 = [T() for _ in range(NS)]
            T16 = [T() for _ in range(NS)]
            sl = [fw.new_sem("pl%d" % i) for i in range(NS)]
            ss = [fw.new_sem("ps%d" % i) for i in range(NS)]
            jobs = []

            def add_job(src, dst, a, b):
                jobs.append((src, dst, a, b))

            for j in range(73):
                c0 = w_in_col0(j)
                wd = 8 if j == 56 else 128
                src = w_in[:, c0:c0 + wd].rearrange("(c p) n -> p c n", p=128)
                add_job(src, win_s[j, :, 0:8 * wd], 8, wd)
            for dc in range(8):
                add_job(w_a[:, dc * 128:(dc + 1) * 128].rearrange("(c p) n -> p c n", p=128), wa_s[dc], 8, 128)
                add_job(w_b[:, dc * 128:(dc + 1) * 128].rearrange("(c p) n -> p c n", p=128), wb_s[dc], 8, 128)
            for dc in range(8):
                add_job(w_o[:, dc * 128:(dc + 1) * 128].rearrange("(c p) n -> p c n", p=128), wo_s[dc], 8, 128)
            for e in range(NE + 1):
                add_job(w_g[e].rearrange("(c p) n -> p c n", p=128), gu_s[e].rearrange("p (c n) -> p c n", c=8)[:, :, 0:256], 8, 256)
                add_job(w_u[e].rearrange("(c p) n -> p c n", p=128), gu_s[e].rearrange("p (c n) -> p c n", c=8)[:, :, 256:512], 8, 256)
                add_job(w_d[e].rearrange("(c p) n -> p c n", p=128), dn_s[e], 2, 1024)
            cast_eng = ["dve", "act", "pool"]

            def job_load(k):
                src_, dst, a, b = jobs[k]
                s = k % NS
                n = a * b
                s32v = stg32[s][:, 0:n].rearrange("p (a b) -> p a b", a=a)
                fw.dma("sp", sl[s], s32v, src_, writes=[T32[s]])

            def job_cast_store(k):
                src_, dst, a, b = jobs[k]
                s = k % NS
                n = a * b
                ce = cast_eng[k % 3]
                if ce == "act":
                    fw.op("act", I.copy(stg16[s][:, 0:n], stg32[s][:, 0:n]), [T32[s]], [T16[s]])
                else:
                    fw.op(ce, I.tensor_copy(stg16[s][:, 0:n], stg32[s][:, 0:n]), [T32[s]], [T16[s]])
                if len(dst.shape) == 3:
                    s16v = stg16[s][:, 0:n].rearrange("p (a b) -> p a b", a=a)
                else:
                    s16v = stg16[s][:, 0:n]
                fw.dma("sp", ss[s], dst, s16v, reads=[T16[s]])

            nj = len(jobs)
            LA = NS - 1
            for k in range(-LA, nj):
                if 0 <= k + LA < nj:
                    job_load(k + LA)
                if k >= 0:
                    job_cast_store(k)
            fw.barrier()
            fw.emit()

        sb = lambda name, shape, dt: st.enter_context(nc.sbuf_tensor("sb_" + name, shape, dt))
        cst = Buf(sb("cst", [128, NCST], F32))
        prm = Buf(sb("prm", [128, NPRM], F32))
        wr32 = Buf(sb("wr32", [128, 8, NE], F32))
        identb = Buf(sb("identb", [128, 128], BF16))
        onesb = Buf(sb("onesb", [128, 128], BF16))
        lbc = Buf(sb("lbc", [128, 16], F32))
        lngb = Buf(sb("lngb", [128, 2 * D], F32))
        S32 = Buf(sb("S32", [128, 8, 128], F32), 8)
        Sb = Buf(sb("Sb", [128, 8, 128], BF16), 8)
        C32 = Buf(sb("C32", [128, 4, 256], F32), 4)
        Cb = Buf(sb("Cb", [128, 4, 256], BF16), 4)
        halo = Buf(sb("halo", [128, 8, 4], F32), 8)
        xt = [Buf(sb("xt%d" % i, [128, D], F32)) for i in range(1)]
        R = Buf(sb("R", [128, 4, D], F32), 4)
        ACC = Buf(sb("ACC", [128, 4, D], F32), 4)
        hT = Buf(sb("hT", [128, 8, TB], BF16), 8)
        hA = Buf(sb("hA", [128, 8, TB], BF16), 8)
        sa16 = [Buf(sb("sa16_%d" % i, [128, TB], BF16)) for i in range(2)]
        yaT = Buf(sb("yaT", [128, 8, TB], BF16), 8)
        ybT = Buf(sb("ybT", [128, 8, TB], BF16), 8)
        mgT = Buf(sb("mgT", [128, 8, TB], BF16), 8)
        big16 = sb("big16", [128, 8, TB], F32)
        BIGT = Buf(big16, 8)
        vtokb_t = big16.bitcast(BF16)
        NF, NBF = 9, 9
        tf = [Buf(sb("tf%d" % i, [128, TB], F32)) for i in range(NF)]
        tb = [Buf(sb("tb%d" % i, [128, TB], BF16)) for i in range(NBF)]
        vtok = Buf(sb("vtok", [64, 8, 128], BF16))
        ktok = Buf(sb("ktok", [64, 8, 128], BF16))
        ATall = Buf(sb("ATall", [64, TB], BF16))
        Sv = [Buf(sb("Sv%d" % i, [128, NCH, 128], BF16)) for i in range(1)]
        svc = {"n": 0}
        Nv = Buf(sb("Nv", [128, NCH, 128], BF16))
        nst = Buf(sb("nst", [128, 24], F32))
        N32c = Buf(sb("N32c", [128, 4], F32))
        xpad = [Buf(sb("xpad%d" % i, [128, TB + 4], F32)) for i in range(1)]
        g4 = {n: Buf(sb("g4_" + n, [4, TB], F32)) for n in ("ig", "f", "P", "rP", "a")}
        lnst = [Buf(sb("lnst%d" % i, [128, 16], F32)) for i in range(2)]
        lnst4 = [Buf(sb("lnst4_%d" % i, [128, 16], F32)) for i in range(4)]
        rt = {n: Buf(sb("rt_" + n, [128, 64], F32)) for n in ("s", "bi", "ms", "sel", "gwu")}
        rt8 = Buf(sb("rt8", [128, 8, 8], F32))
        rts = Buf(sb("rts", [128, 40], F32))
        rtd = Buf(sb("rtd", [128, 2], F32))
        gw = Buf(sb("gw", [128, 8, NE], F32), 8)
        GU = [Buf(sb("GU%d" % i, [128, 8, 512], BF16)) for i in range(2)]
        DN = [Buf(sb("DN%d" % i, [128, 2, 1024], BF16)) for i in range(2)]
        GT = [Buf(sb("GT%d" % i, [128, 2, TB], BF16), 2) for i in range(2)]
        WT = [Buf(sb("WT%d" % i, [128, 8, 128], BF16)) for i in range(3)]
        print("sbuf bytes remaining:", nc.sbuf_bytes_remaining)
        banks = [Buf(st.enter_context(nc.psum_tensor("bank%d" % i, [128, 512], F32))) for i in range(8)]
        free_banks = list(range(4))
        free_banks_m = list(range(4, 8))
        for b_ in banks:
            b_.T.excl = True

        def pget():
            return banks[free_banks.pop(0)]

        def pput(b):
            k_ = banks.index(b)
            (free_banks if k_ < 4 else free_banks_m).append(k_)

        def mget():
            return banks[free_banks_m.pop(0)]

        s_x = [fw.new_sem("sx%d" % i) for i in range(2)]
        s_y = [fw.new_sem("sy%d" % i) for i in range(4)]
        s_wt = [fw.new_sem("swt%d" % i) for i in range(4)]
        s_gu = [fw.new_sem("sgu%d" % i) for i in range(2)]
        s_dn = [fw.new_sem("sdn%d" % i) for i in range(2)]
        s_ln = fw.new_sem("sln")
        s_c = fw.new_sem("sc")
        s_c2 = fw.new_sem("sc2")
        s_c3 = fw.new_sem("sc3")
        s_tap = fw.new_sem("stap")

        fw.dma("sp", s_c, cst.t[:], cst_d, writes=[cst.T])
        fw.dma("sp", s_c2, prm.t[:], prm_d, writes=[prm.T])
        fw.dma("sp", s_c3, wr32.t[:], w_r.rearrange("(c p) n -> p c n", p=128), writes=[wr32.T])
        V(I.tensor_copy(identb.t[:], cst.t[:, C_ID:C_ID + 128]), [cst.T], [identb.T])
        V(I.tensor_copy(onesb.t[:], cst.t[:, C_ONES:C_ONES + 128]), [cst.T], [onesb.T])
        V(I.tensor_tensor(lbc.t[:, 0:8], prm.t[:, P_LB0:P_LB0 + 8], prm.t[:, P_LB1:P_LB1 + 8], ALU.subtract), [prm.T], [lbc.T])
        A(I.activation(lbc.t[:, 0:8], lbc.t[:, 0:8], AF.Sigmoid), [lbc.T], [lbc.T])
        V(I.tensor_scalar(lbc.t[:, 8:16], lbc.t[:, 0:8], -1.0, 1.0, ALU.mult, ALU.add), [lbc.T], [lbc.T])
        for bufz in (S32, C32, N32c, halo):
            G(I.memset(bufz.t[:], 0.0), [], bufz.Ts)
        for bufz in (Sb, Cb):
            G(I.memset(bufz.t[:], 0.0), [], bufz.Ts)

        wt_state = {"n": 0}

        def load_wt(src_ap, ncol=128):
            k = wt_state["n"] % 3
            wt_state["n"] += 1
            b = WT[k]
            fw.dma("sp", s_wt[k], b.t[:, :, 0:ncol], src_ap.rearrange("p (c n) -> p c n", c=8), writes=[b.T])
            return b

        def win_tile(j):
            if j == 56:
                return load_wt(win_s[j, :, 0:64], 8)
            return load_wt(win_s[j])

        def proj_fm(bank, wt, ncol=128, m0=0):
            MM([I.matmul(bank.t[0:ncol, :], wt.t[:, c, m0:m0 + ncol], hA.t[:, c, :],
                                        start=(c == 0), stop=(c == 7)) for c in range(8)],
               [wt.T] + hA.Ts, [bank.T])

        lncnt = {"n": 0}

        def layer_norm(xap, xT, oap=None, oT=None):
            if oap is None:
                oap, oT = xap, xT
            k = lncnt["n"] % 2
            lncnt["n"] += 1
            s = lnst[k]
            V(I.bn_stats(s.t[:, 0:6], xap[:, 0:512]), [xT], [s.T])
            V(I.bn_stats(s.t[:, 6:12], xap[:, 512:1024]), [xT], [s.T])
            V(I.bn_aggr(s.t[:, 12:14], s.t[:, 0:12]), [s.T], [s.T])
            A(I.activation(s.t[:, 14:15], s.t[:, 13:14], AF.Sqrt, bias=float(EPS)), [s.T], [s.T])
            V(I.reciprocal(s.t[:, 14:15], s.t[:, 14:15]), [s.T], [s.T])
            V(I.tensor_scalar(oap, xap, s.t[:, 12:13], s.t[:, 14:15], ALU.subtract, ALU.mult), [xT, s.T], [oT])
            V(I.tensor_tensor(oap, oap, lngb.t[:, 0:D], ALU.mult), [oT, lngb.T], [oT])
            G(I.tensor_tensor(oap, oap, lngb.t[:, D:2 * D], ALU.add), [oT, lngb.T], [oT])

        def transpose_to_fm(src, dst32, dstb, dstTs_b, dstTs_32):
            for i in range(4):
                for g in range(2):
                    bk = pget()
                    MM([I.transpose(bk.t[:, (c % 4) * 128:(c % 4 + 1) * 128],
                                                   src.t[:, i, c * 128:(c + 1) * 128], cst.t[:, C_ID:C_ID + 128])
                        for c in range(4 * g, 4 * g + 4)], [src.Ts[i], cst.T], [bk.T])
                    bv = bk.t[:, :].rearrange("p (c t) -> p c t", c=4)
                    if dstb is not None:
                        A(I.copy(dstb[:, 4 * g:4 * g + 4, i * 128:(i + 1) * 128], bv),
                          [bk.T], [dstTs_b[g * 4 + i]])
                    if dst32 is not None:
                        if g == 0:
                            V(I.tensor_copy(dst32[:, 4 * g:4 * g + 4, i * 128:(i + 1) * 128], bv),
                              [bk.T], [dstTs_32[g * 4 + i]])
                        else:
                            A(I.copy(dst32[:, 4 * g:4 * g + 4, i * 128:(i + 1) * 128], bv),
                              [bk.T], [dstTs_32[g * 4 + i]])
                    pput(bk)

        def bc_last(buf_t, nparts, nrep, col0=63):
            base = buf_t[0:nparts, 0:1]
            return bass.AP(base.tensor, base.offset + col0, [[base.ap[0][0], nparts], [CH, NCH], [0, nrep]])

        _b = cst.t[:, C_NH:C_NH + 1]
        nh512 = bass.AP(_b.tensor, _b.offset, [[_b.ap[0][0], 128], [0, TB]])

        _m = cst.t[0:64, C_MASK:C_MASK + 1]
        mask_bc = bass.AP(_m.tensor, _m.offset, [[_m.ap[0][0], 64], [0, NCH], [1, CH]])

        def sv32(ch):
            return tf[7 + ch // 4].t[:, (ch % 4) * 128:(ch % 4 + 1) * 128]

        def v3(ap2d, a):
            return ap2d.rearrange("p (a b) -> p a b", a=a)

        def tap(name, ap, Ts):
            if name in tap_out:
                fw.dma("sp", s_tap, tap_out[name], ap, reads=Ts)

        def gen_front(blk):
            first = (blk == 0)
            fw.dma("sp", s_ln, lngb.t[:], lnp_d[0], writes=[lngb.T])
            for i in range(4):
                xb_ = xt[0]
                r0 = blk * TB + i * 128
                fw.dma("sp", s_x[0], xb_.t[:], xin[r0:r0 + 128, :], writes=[xb_.T])
                A(I.copy(R.t[:, i, :], xb_.t[:]), [xb_.T], [R.Ts[i]])
                layer_norm(R.t[:, i, :], R.Ts[i])
                yield 2
            transpose_to_fm(R, None, hA.t, hA.Ts, None)
            yield 4
            if first:
                tap("h", R.t[:, 0, :], [R.Ts[0]])

            for hd in range(8):
                p = 0
                F = lambda k: tf[p * 7 + k]
                Bq = lambda k: tb[p * 6 + k]
                fgt, kk, Pt, rP, nt, rstd, t1 = [F(k) for k in range(7)]
                kt, qt, qh, sg, sq, _ = [Bq(k) for k in range(6)]
                wq = win_tile(hd)
                bq = pget(); proj_fm(bq, wq)
                yield 1
                wf = win_tile(8 + hd)
                bf_ = pget(); proj_fm(bf_, wf)
                yield 1
                wg_ = win_tile(24 + hd)
                bg = pget(); proj_fm(bg, wg_)
                yield 1
                wv = win_tile(16 + hd)
                bvf = pget(); proj_fm(bvf, wv)
                vfm = tb[6]
                A(I.copy(vfm.t[:], bvf.t[:]), [bvf.T], [vfm.T])
                pput(bvf)
                yield 2
                bvT = pget()
                bvT16 = bvT.t.bitcast(BF16)
                MM([I.transpose(bvT16[0:64, ch * 128:(ch + 1) * 128], vfm.t[:, ch * 64:(ch + 1) * 64], identb.t[:])
                    for ch in range(NCH)], [vfm.T, identb.T], [bvT.T])
                A(I.copy(vtok.t[:, :, :], v3(bvT16[0:64, 0:1024], 8)), [bvT.T], [vtok.T])
                pput(bvT)
                yield 2
                A(I.activation(fgt.t[:], bf_.t[:], AF.Sigmoid), [bf_.T], [fgt.T])
                pput(bf_)
                A(I.activation(sg.t[:], bg.t[:], AF.Silu), [bg.T], [sg.T])
                pput(bg)
                V(I.tensor_scalar(fgt.t[:], fgt.t[:], lbc.t[:, 8 + hd:9 + hd], lbc.t[:, hd:hd + 1], ALU.mult, ALU.add),
                  [fgt.T, lbc.T], [fgt.T])
                V(I.tensor_scalar(kk.t[:], fgt.t[:], -1.0, 1.0, ALU.mult, ALU.add), [fgt.T], [kk.T])
                yield 1
                for ch in range(NCH):
                    V(I.tensor_tensor_scan(Pt.t[:, ch * 64:(ch + 1) * 64], fgt.t[:, ch * 64:(ch + 1) * 64],
                                                            cst.t[:, C_ZERO:C_ZERO + 64], 1.0, ALU.mult, ALU.add),
                      [fgt.T, cst.T], [Pt.T])
                V(I.reciprocal(rP.t[:], Pt.t[:]), [Pt.T], [rP.T])
                yield 1
                V(I.tensor_tensor(kk.t[:], kk.t[:], rP.t[:], ALU.mult), [kk.T, rP.T], [kk.T])
                V(I.tensor_tensor(v3(kt.t[:], NCH), v3(kk.t[:], NCH), bc_last(Pt.t, 128, CH), ALU.mult), [kk.T, Pt.T], [kt.T])
                yield 1
                V(I.tensor_tensor(fgt.t[:], bq.t[:], Pt.t[:], ALU.mult), [bq.T, Pt.T], [fgt.T])
                pput(bq)
                A(I.copy(qh.t[:], fgt.t[:]), [fgt.T], [qh.T])
                V(I.tensor_tensor(v3(qt.t[:], NCH), v3(fgt.t[:], NCH), bc_last(rP.t, 128, CH), ALU.mult), [fgt.T, rP.T], [qt.T])
                yield 1
                yield 4
                bkT = pget()
                bkT16 = bkT.t.bitcast(BF16)
                MM([I.transpose(bkT16[0:64, ch * 128:(ch + 1) * 128], kt.t[:, ch * 64:(ch + 1) * 64], identb.t[:])
                    for ch in range(NCH)], [kt.T, identb.T], [bkT.T])
                A(I.copy(ktok.t[:, :, :], v3(bkT16[0:64, 0:1024], 8)), [bkT.T], [ktok.T])
                pput(bkT)
                yield 2
                bsc = pget()
                MM([I.matmul(bsc.t[0:64, ch * 64:(ch + 1) * 64], kt.t[:, ch * 64:(ch + 1) * 64], qt.t[:, ch * 64:(ch + 1) * 64],
                             start=True, stop=True) for ch in range(NCH)], [kt.T, qt.T], [bsc.T])
                yield 1
                V(I.tensor_tensor(v3(ATall.t[:, :], NCH), v3(bsc.t[0:64, :], NCH), mask_bc, ALU.mult), [bsc.T, cst.T], [ATall.T])
                pput(bsc)
                bds = [pget(), pget()]
                for hb_ in range(2):
                    MM([I.matmul(bds[hb_].t[:, (ch % 4) * 128:(ch % 4 + 1) * 128], ktok.t[:, ch, :], vtok.t[:, ch, :], start=True, stop=True)
                        for ch in range(hb_ * 4, hb_ * 4 + 4)], [ktok.T, vtok.T], [bds[hb_].T])
                sv = Sv[0]
                svc["n"] += 1
                for ch in range(NCH):
                    pl = Pt.t[:, ch * 64 + 63:ch * 64 + 64]
                    bsl = bds[ch // 4].t[:, (ch % 4) * 128:(ch % 4 + 1) * 128]
                    prev = S32.t[:, hd, :] if ch == 0 else sv32(ch - 1)
                    V(I.scalar_tensor_tensor(sv32(ch), prev, pl, bsl, ALU.mult, ALU.add),
                      [S32.Ts[hd], Pt.T, bds[ch // 4].T, tf[7].T, tf[8].T], [tf[7 + ch // 4].T])
                    yield 1
                pput(bds[0]); pput(bds[1])
                A(I.copy(sv.t[:, 0:4, :], v3(tf[7].t[:], 4)), [tf[7].T], [sv.T])
                A(I.copy(sv.t[:, 4:8, :], v3(tf[8].t[:], 4)), [tf[8].T], [sv.T])
                G(I.tensor_copy(S32.t[:, hd, :], sv32(NCH - 1)), [tf[8].T], [S32.Ts[hd]])
                yield 8
                bo = pget()
                fns = []
                for ch in range(NCH):
                    cs = slice(ch * 64, (ch + 1) * 64)
                    sprev = Sb.t[:, hd, :] if ch == 0 else sv.t[:, ch - 1, :]
                    fns.append(I.matmul(bo.t[:, cs], vtok.t[:, ch, :], ATall.t[:, cs], start=True, stop=False))
                    fns.append(I.matmul(bo.t[:, cs], sprev, qh.t[:, cs], start=False, stop=True))
                MM(fns, [vtok.T, ATall.T, Sb.Ts[hd], sv.T, qh.T], [bo.T])
                G(I.tensor_copy(Sb.t[:, hd, :], sv.t[:, NCH - 1, :]), [sv.T], [Sb.Ts[hd]])
                yield 2
                A(I.activation(sq.t[:], bo.t[:], AF.Square), [bo.T], [sq.T])
                A(I.copy(t1.t[:], bo.t[:]), [bo.T], [t1.T])
                pput(bo)
                yield 3
                bm = pget()
                MM([I.matmul(bm.t[:], onesb.t[:], sq.t[:], start=True, stop=True)], [onesb.T, sq.T], [bm.T])
                yield 2
                A(I.activation(nt.t[:], bm.t[:], AF.Sqrt, bias=float(EPS), scale=1.0 / 128.0), [bm.T], [nt.T])
                pput(bm)
                V(I.reciprocal(rstd.t[:], nt.t[:]), [nt.T], [rstd.T])
                V(I.tensor_tensor(t1.t[:], t1.t[:], rstd.t[:], ALU.mult), [t1.T, rstd.T], [t1.T])
                yield 1
                V(I.scalar_tensor_tensor(yaT.t[:, hd, :], t1.t[:], prm.t[:, P_HGG + hd:P_HGG + hd + 1], sg.t[:], ALU.mult, ALU.mult),
                  [t1.T, prm.T, sg.T], [yaT.Ts[hd]])

            wgt = win_tile(56)
            big_ = pget(); proj_fm(big_, wgt, 4, 0)
            bfg = pget(); proj_fm(bfg, wgt, 4, 4)
            A(I.activation(g4["ig"].t[:], big_.t[0:4, :], AF.Exp, bias=prm.t[0:4, P_IGB:P_IGB + 1]), [big_.T, prm.T], [g4["ig"].T])
            A(I.activation(g4["f"].t[:], bfg.t[0:4, :], AF.Sigmoid, bias=prm.t[0:4, P_FGB:P_FGB + 1]), [bfg.T, prm.T], [g4["f"].T])
            pput(big_); pput(bfg)
            yield 4
            for ch in range(NCH):
                V(I.tensor_tensor_scan(g4["P"].t[:, ch * 64:(ch + 1) * 64], g4["f"].t[:, ch * 64:(ch + 1) * 64],
                                                        cst.t[0:4, C_ZERO:C_ZERO + 64], 1.0, ALU.mult, ALU.add),
                  [g4["f"].T, cst.T], [g4["P"].T])
                yield 1
            V(I.reciprocal(g4["rP"].t[:], g4["P"].t[:]), [g4["P"].T], [g4["rP"].T])
            V(I.tensor_tensor(g4["a"].t[:], g4["ig"].t[:], g4["rP"].t[:], ALU.mult), [g4["ig"].T, g4["rP"].T], [g4["a"].T])
            yield 1
            for hd in range(4):
                abc, pbc, dens, wsc, h0, h1_, mean, m2, var = [tf[k] for k in range(9)]
                qb, kb, khat, so0, so1, hs0, hs1, hb0, hb1 = [tb[k] for k in range(9)]
                for src, dstb in ((g4["a"], abc), (g4["P"], pbc)):
                    bb = pget()
                    MM([I.matmul(bb.t[:], cst.t[0:4, C_SEL + hd * 128:C_SEL + (hd + 1) * 128], src.t[:],
                                                                  start=True, stop=True)], [cst.T, src.T], [bb.T])
                    A(I.copy(dstb.t[:], bb.t[:]), [bb.T], [dstb.T])
                    pput(bb)
                    yield 1
                for which, dst in ((0, qb), (1, kb)):
                    j = 32 + which * 4 + hd
                    cidx = which * 4 + hd
                    wq = win_tile(j)
                    bq = pget(); proj_fm(bq, wq)
                    xp = xpad[0]
                    A(I.copy(xp.t[:, 4:4 + TB], bq.t[:]), [bq.T], [xp.T])
                    pput(bq)
                    yield 1
                    V(I.tensor_copy(xp.t[:, 0:4], halo.t[:, cidx, :]), [halo.Ts[cidx]], [xp.T])
                    V(I.tensor_copy(halo.t[:, cidx, :], xp.t[:, TB:TB + 4]), [xp.T], [halo.Ts[cidx]])
                    yield 1
                    cv = tf[7 + which]
                    V(I.tensor_scalar(cv.t[:], xp.t[:, 1:1 + TB], prm.t[:, P_CW + cidx:P_CW + cidx + 1],
                                                                        prm.t[:, P_CB + cidx:P_CB + cidx + 1], ALU.mult, ALU.add),
                      [xp.T, prm.T], [cv.T])
                    for tp in range(1, 4):
                        V(I.scalar_tensor_tensor(cv.t[:], xp.t[:, 1 + tp:1 + tp + TB],
                                                                                          prm.t[:, P_CW + tp * 8 + cidx:P_CW + tp * 8 + cidx + 1],
                                                                                          cv.t[:], ALU.mult, ALU.add),
                          [xp.T, prm.T, cv.T], [cv.T])
                        yield 1
                    A(I.activation(dst.t[:], cv.t[:], AF.Silu), [cv.T], [dst.T])
                V(I.tensor_scalar(qb.t[:], qb.t[:], 128.0 ** -0.5, None, ALU.mult), [qb.T], [qb.T])
                V(I.tensor_tensor(khat.t[:], kb.t[:], abc.t[:], ALU.mult), [kb.T, abc.T], [khat.T])
                yield 1
                for vt, so in ((0, so0), (1, so1)):
                    wq = win_tile(48 + hd * 2 + vt)
                    bq = pget(); proj_fm(bq, wq)
                    A(I.activation(so.t[:], bq.t[:], AF.Sigmoid), [bq.T], [so.T])
                    pput(bq)
                    yield 1
                vtb = vtokb_t
                for vt in range(2):
                    wv = win_tile(40 + hd * 2 + vt)
                    bvf = pget(); proj_fm(bvf, wv)
                    vfm = hs0
                    A(I.copy(vfm.t[:], bvf.t[:]), [bvf.T], [vfm.T])
                    pput(bvf)
                    yield 2
                    bvT = pget()
                    bvT16 = bvT.t.bitcast(BF16)
                    MM([I.transpose(bvT16[0:64, ch * 128:(ch + 1) * 128], vfm.t[:, ch * 64:(ch + 1) * 64], identb.t[:])
                        for ch in range(NCH)], [vfm.T, identb.T], [bvT.T])
                    A(I.copy(vtb[0:64, :, vt * 128:(vt + 1) * 128], v3(bvT16[0:64, 0:1024], 8)), [bvT.T], BIGT.Ts)
                    pput(bvT)
                    yield 2
                yield 3
                bkT = pget()
                bkT16 = bkT.t.bitcast(BF16)
                MM([I.transpose(bkT16[0:64, ch * 128:(ch + 1) * 128], khat.t[:, ch * 64:(ch + 1) * 64], identb.t[:])
                    for ch in range(NCH)], [khat.T, identb.T], [bkT.T])
                V(I.tensor_tensor(ktok.t[:, :, :], v3(bkT16[0:64, 0:1024], 8), bc_last(pbc.t, 64, 128), ALU.mult),
                  [bkT.T, pbc.T], [ktok.T])
                pput(bkT)
                yield 2
                bsc = pget()
                MM([I.matmul(bsc.t[0:64, ch * 64:(ch + 1) * 64], khat.t[:, ch * 64:(ch + 1) * 64], qb.t[:, ch * 64:(ch + 1) * 64],
                             start=True, stop=True) for ch in range(NCH)], [khat.T, qb.T], [bsc.T])
                yield 1
                V(I.tensor_tensor(v3(ATall.t[:, :], NCH), v3(bsc.t[0:64, :], NCH), mask_bc, ALU.mult), [bsc.T, cst.T], [ATall.T])
                yield 1
                pput(bsc)
                V(I.tensor_reduce(nst.t[:, 16:24], v3(khat.t[:], NCH), mybir.AxisListType.X, ALU.add), [khat.T], [nst.T])
                V(I.tensor_copy(nst.t[:, 0:1], N32c.t[:, hd:hd + 1]), [N32c.T], [nst.T])
                yield 1
                for ch in range(NCH):
                    V(I.scalar_tensor_tensor(nst.t[:, ch + 1:ch + 2], nst.t[:, ch:ch + 1], nst.t[:, 16 + ch:17 + ch],
                                             pbc.t[:, ch * 64 + 63:ch * 64 + 64], ALU.add, ALU.mult), [nst.T, pbc.T], [nst.T])
                V(I.tensor_copy(N32c.t[:, hd:hd + 1], nst.t[:, 8:9]), [nst.T], [N32c.T])
                yield 1
                _nb = nst.t[:, 0:1]
                V(I.tensor_copy(Nv.t[:, :, :], bass.AP(_nb.tensor, _nb.offset, [[_nb.ap[0][0], 128], [1, NCH], [0, 128]])), [nst.T], [Nv.T])
                yield 3
                bdn = pget()
                fns = []
                for ch in range(NCH):
                    cs = slice(ch * 64, (ch + 1) * 64)
                    fns.append(I.matmul(bdn.t[:, cs], onesb.t[0:64, :], ATall.t[:, cs], start=True, stop=False))
                    fns.append(I.matmul(bdn.t[:, cs], Nv.t[:, ch, :], qb.t[:, cs], start=False, stop=True))
                MM(fns, [onesb.T, ATall.T, Nv.T, qb.T], [bdn.T])
                V(I.tensor_tensor(dens.t[:], bdn.t[:], pbc.t[:], ALU.mult), [bdn.T, pbc.T], [dens.T])
                yield 1
                pput(bdn)
                bns = []
                for vt in range(2):
                    vsl = slice(vt * 128, (vt + 1) * 128)
                    bds = [pget(), pget()]
                    for hb_ in range(2):
                        MM([I.matmul(bds[hb_].t[:, (ch % 4) * 128:(ch % 4 + 1) * 128], ktok.t[:, ch, :], vtb[0:64, ch, vsl], start=True, stop=True)
                            for ch in range(hb_ * 4, hb_ * 4 + 4)], [ktok.T] + BIGT.Ts, [bds[hb_].T])
                    sv = Sv[0]
                    svc["n"] += 1
                    for ch in range(NCH):
                        pl = pbc.t[:, ch * 64 + 63:ch * 64 + 64]
                        bsl = bds[ch // 4].t[:, (ch % 4) * 128:(ch % 4 + 1) * 128]
                        prev = C32.t[:, hd, vsl] if ch == 0 else sv32(ch - 1)
                        V(I.scalar_tensor_tensor(sv32(ch), prev, pl, bsl, ALU.mult, ALU.add),
                          [C32.Ts[hd], pbc.T, bds[ch // 4].T, tf[7].T, tf[8].T], [tf[7 + ch // 4].T])
                    pput(bds[0]); pput(bds[1])
                    A(I.copy(sv.t[:, 0:4, :], v3(tf[7].t[:], 4)), [tf[7].T], [sv.T])
                    A(I.copy(sv.t[:, 4:8, :], v3(tf[8].t[:], 4)), [tf[8].T], [sv.T])
                    G(I.tensor_copy(C32.t[:, hd, vsl], sv32(NCH - 1)), [tf[8].T], [C32.Ts[hd]])
                    yield 8
                    bn = pget()
                    fns = []
                    for ch in range(NCH):
                        cs = slice(ch * 64, (ch + 1) * 64)
                        cprev = Cb.t[:, hd, vsl] if ch == 0 else sv.t[:, ch - 1, :]
                        fns.append(I.matmul(bn.t[:, cs], vtb[0:64, ch, vsl], ATall.t[:, cs], start=True, stop=False))
                        fns.append(I.matmul(bn.t[:, cs], cprev, qb.t[:, cs], start=False, stop=True))
                    MM(fns, BIGT.Ts + [ATall.T, Cb.Ts[hd], sv.T, qb.T], [bn.T])
                    G(I.tensor_copy(Cb.t[:, hd, vsl], sv.t[:, NCH - 1, :]), [sv.T], [Cb.Ts[hd]])
                    hh_ = (h0, h1_)[vt]
                    A(I.copy(hh_.t[:], bn.t[:]), [bn.T], [hh_.T])
                    pput(bn)
                    yield 2
                A(I.activation(dens.t[:], dens.t[:], AF.Abs), [dens.T], [dens.T])
                V(I.tensor_scalar(dens.t[:], dens.t[:], 1.0, None, ALU.max), [dens.T], [dens.T])
                yield 1
                V(I.reciprocal(dens.t[:], dens.t[:]), [dens.T], [dens.T])
                V(I.tensor_tensor(wsc.t[:], pbc.t[:], dens.t[:], ALU.mult), [pbc.T, dens.T], [wsc.T])
                yield 1
                for hh, hs, hb in ((h0, hs0, hb0), (h1_, hs1, hb1)):
                    V(I.tensor_tensor(hh.t[:], hh.t[:], wsc.t[:], ALU.mult), [hh.T, wsc.T], [hh.T])
                    A(I.activation(hs.t[:], hh.t[:], AF.Square), [hh.T], [hs.T])
                    A(I.copy(hb.t[:], hh.t[:]), [hh.T], [hb.T])
                yield 8
                bsu = pget(); bsq = pget()
                MM([I.matmul(bsu.t[:], onesb.t[:], hb0.t[:], start=True, stop=False),
                    I.matmul(bsu.t[:], onesb.t[:], hb1.t[:], start=False, stop=True)], [onesb.T, hb0.T, hb1.T], [bsu.T])
                MM([I.matmul(bsq.t[:], onesb.t[:], hs0.t[:], start=True, stop=False),
                    I.matmul(bsq.t[:], onesb.t[:], hs1.t[:], start=False, stop=True)], [onesb.T, hs0.T, hs1.T], [bsq.T])
                A(I.activation(mean.t[:], bsu.t[:], AF.Identity, scale=1.0 / 256.0), [bsu.T], [mean.T])
                pput(bsu)
                V(I.tensor_tensor(m2.t[:], mean.t[:], mean.t[:], ALU.mult), [mean.T], [m2.T])
                yield 1
                V(I.scalar_tensor_tensor(var.t[:], bsq.t[:], 1.0 / 256.0, m2.t[:], ALU.mult, ALU.subtract), [bsq.T, m2.T], [var.T])
                pput(bsq)
                A(I.activation(var.t[:], var.t[:], AF.Sqrt, bias=float(EPS)), [var.T], [var.T])
                V(I.reciprocal(var.t[:], var.t[:]), [var.T], [var.T])
                yield 1
                for vt, hh, so in ((0, h0, so0), (1, h1_, so1)):
                    V(I.tensor_tensor(hh.t[:], hh.t[:], mean.t[:], ALU.subtract), [hh.T, mean.T], [hh.T])
                    V(I.tensor_tensor(hh.t[:], hh.t[:], var.t[:], ALU.mult), [hh.T, var.T], [hh.T])
                    yield 1
                    cg = P_MLG + hd * 2 + vt
                    V(I.scalar_tensor_tensor(ybT.t[:, hd * 2 + vt, :], hh.t[:], prm.t[:, cg:cg + 1], so.t[:], ALU.mult, ALU.mult),
                      [hh.T, prm.T, so.T], [ybT.Ts[hd * 2 + vt]])

            for dc in range(8):
                wa = load_wt(wa_s[dc])
                ba = pget()
                MM([I.matmul(ba.t[:], wa.t[:, c, :], yaT.t[:, c, :], start=(c == 0), stop=(c == 7)) for c in range(8)],
                   [wa.T] + yaT.Ts, [ba.T])
                wbt = load_wt(wb_s[dc])
                bb = pget()
                MM([I.matmul(bb.t[:], wbt.t[:, c, :], ybT.t[:, c, :], start=(c == 0), stop=(c == 7)) for c in range(8)],
                   [wbt.T] + ybT.Ts, [bb.T])
                wga = win_tile(57 + dc)
                yield 2
                bga = pget(); proj_fm(bga, wga)
                wgb = win_tile(65 + dc)
                bgb = pget(); proj_fm(bgb, wgb)
                sga, sgb, m1 = tf[0], tf[1], tf[2]
                A(I.activation(sga.t[:], bga.t[:], AF.Sigmoid), [bga.T], [sga.T])
                A(I.activation(sgb.t[:], bgb.t[:], AF.Sigmoid), [bgb.T], [sgb.T])
                pput(bga); pput(bgb)
                V(I.tensor_tensor(m1.t[:], ba.t[:], sga.t[:], ALU.mult), [ba.T, sga.T], [m1.T])
                yield 1
                V(I.tensor_tensor(sgb.t[:], bb.t[:], sgb.t[:], ALU.mult), [bb.T, sgb.T], [sgb.T])
                pput(ba); pput(bb)
                V(I.tensor_tensor(mgT.t[:, dc, :], m1.t[:], sgb.t[:], ALU.add), [m1.T, sgb.T], [mgT.Ts[dc]])
                yield 1
                yield 2

            fw.dma("sp", s_ln, lngb.t[:], lnp_d[1], writes=[lngb.T])
            for hf in range(2):
                bks = [pget() for _ in range(4)]
                for dcl in range(4):
                    wt = load_wt(wo_s[hf * 4 + dcl])
                    for i in range(4):
                        MM([I.matmul(bks[i].t[:, dcl * 128:(dcl + 1) * 128], mgT.t[:, c, i * 128:(i + 1) * 128], wt.t[:, c, :],
                                     start=(c == 0), stop=(c == 7)) for c in range(8)], mgT.Ts + [wt.T], [bks[i].T])
                    yield 2
                for i in range(4):
                    V(I.scalar_tensor_tensor(R.t[:, i, hf * 512:(hf + 1) * 512], R.t[:, i, hf * 512:(hf + 1) * 512],
                                             float(ALPHA), bks[i].t[:], ALU.mult, ALU.add), [R.Ts[i], bks[i].T], [R.Ts[i]])
                    pput(bks[i])
                yield 1
            for i in range(4):
                layer_norm(R.t[:, i, :], R.Ts[i])
                yield 1
            if first:
                tap("h1", R.t[:, 0, :], [R.Ts[0]])

            transpose_to_fm(R, big16, None, None, BIGT.Ts)
            yield 3
            par = blk % 2
            for i in range(4):
                bk = pget()
                MM([I.matmul(bk.t[:, 0:NE], big16[:, c, i * 128:(i + 1) * 128], wr32.t[:, c, :],
                                                       start=(c == 0), stop=(c == 7)) for c in range(8)],
                   BIGT.Ts + [wr32.T], [bk.T])
                s_, bi, ms_, sel, gwu = rt["s"], rt["bi"], rt["ms"], rt["sel"], rt["gwu"]
                A(I.activation(s_.t[:], bk.t[:, 0:NE], AF.Sigmoid), [bk.T], [s_.T])
                pput(bk)
                V(I.tensor_tensor(bi.t[:], s_.t[:], cst.t[:, C_RB:C_RB + NE], ALU.add), [s_.T, cst.T], [bi.T])
                yield 1
                for g in range(8):
                    V(I.max(rt8.t[:, g, :], bi.t[:, g * 8:(g + 1) * 8]), [bi.T], [rt8.T])
                V(I.tensor_tensor(rts.t[:, 0:8], rt8.t[:, :, 0], rt8.t[:, :, 1], ALU.add), [rt8.T], [rts.T])
                yield 1
                V(I.max(rts.t[:, 8:16], rts.t[:, 0:8]), [rts.T], [rts.T])
                V(I.tensor_scalar(rts.t[:, 16:24], rts.t[:, 0:8], rts.t[:, 11:12], None, ALU.is_ge), [rts.T], [rts.T])
                yield 1
                V(I.tensor_scalar(rts.t[:, 24:32], rts.t[:, 16:24], BIG, -BIG, ALU.mult, ALU.add), [rts.T], [rts.T])


                def gb8(c0):
                    base = rts.t[:, c0:c0 + 1]
                    return bass.AP(base.tensor, base.offset, [[base.ap[0][0], 128], [1, 8], [0, 8]])
                V(I.tensor_tensor(v3(ms_.t[:], 8), v3(bi.t[:], 8), gb8(16), ALU.mult), [bi.T, rts.T], [ms_.T])
                yield 1
                V(I.tensor_tensor(v3(ms_.t[:], 8), v3(ms_.t[:], 8), gb8(24), ALU.add), [ms_.T, rts.T], [ms_.T])
                V(I.max(rts.t[:, 32:40], ms_.t[:]), [ms_.T], [rts.T])
                yield 1
                V(I.tensor_scalar(sel.t[:], ms_.t[:], rts.t[:, 39:40], None, ALU.is_ge), [ms_.T, rts.T], [sel.T])
                V(I.tensor_tensor(gwu.t[:], s_.t[:], sel.t[:], ALU.mult), [s_.T, sel.T], [gwu.T])
                yield 1
                V(I.tensor_reduce(rtd.t[:, 0:1], gwu.t[:], mybir.AxisListType.X, ALU.add), [gwu.T], [rtd.T])
                V(I.reciprocal(rtd.t[:, 1:2], rtd.t[:, 0:1]), [rtd.T], [rtd.T])
                yield 1
                V(I.tensor_scalar(gw.t[:, par * 4 + i, :], gwu.t[:], rtd.t[:, 1:2], 2.5, ALU.mult, ALU.mult), [gwu.T, rtd.T], [gw.Ts[par * 4 + i]])
                yield 2
            if first:
                tap("gw", gw.t[:, 0, :], [gw.Ts[0]])

        def gen_moe(blk):
            par = blk % 2

            def load_gu(e):
                k = e % 2
                fw.dma("sp", s_gu[k], GU[k].t[:], gu_s[e].rearrange("p (c n) -> p c n", c=8), writes=[GU[k].T])

            def load_dn(e):
                k = e % 2
                fw.dma("sp", s_dn[k], DN[k].t[:], dn_s[e].rearrange("p (c n) -> p c n", c=2), writes=[DN[k].T])

            def gate_up(e, ft):
                k = e % 2
                gu, gt = GU[k], GT[k]
                bga = mget()
                MM([I.matmul(bga.t[:], gu.t[:, c, ft * 128:(ft + 1) * 128], hT.t[:, c, :],
                             start=(c == 0), stop=(c == 7)) for c in range(8)], [gu.T] + hT.Ts, [bga.T])
                bup = mget()
                MM([I.matmul(bup.t[:], gu.t[:, c, 256 + ft * 128:256 + (ft + 1) * 128], hT.t[:, c, :],
                             start=(c == 0), stop=(c == 7)) for c in range(8)], [gu.T] + hT.Ts, [bup.T])
                sa = sa16[ft]
                A(I.activation(sa.t[:], bga.t[:], AF.Silu), [bga.T], [sa.T])
                pput(bga)
                V(I.tensor_tensor(gt.t[:, ft, :], bup.t[:], sa.t[:], ALU.mult), [bup.T, sa.T], [gt.Ts[ft]])
                pput(bup)

            def down(e, i, hf):
                k = e % 2
                dn, gt = DN[k], GT[k]
                bk = mget()
                MM([I.matmul(bk.t[:], gt.t[:, kk_, i * 128:(i + 1) * 128], dn.t[:, kk_, hf * 512:(hf + 1) * 512],
                             start=(kk_ == 0), stop=(kk_ == 1)) for kk_ in range(2)], gt.Ts + [dn.T], [bk.T])
                acc = ACC.t[:, i, hf * 512:(hf + 1) * 512]
                if e < NE:
                    V(I.scalar_tensor_tensor(acc, bk.t[:], gw.t[:, par * 4 + i, e:e + 1], acc, ALU.mult, ALU.add),
                      [bk.T, gw.Ts[par * 4 + i], ACC.Ts[i]], [ACC.Ts[i]])
                else:
                    V(I.tensor_tensor(acc, bk.t[:], acc, ALU.add), [bk.T, ACC.Ts[i]], [ACC.Ts[i]])
                pput(bk)

            load_gu(0)
            load_dn(0)
            load_gu(1)
            for e in range(NE + 2):
                if e <= NE:
                    gate_up(e, 0)
                    yield 1
                if e >= 1:
                    for i in range(2):
                        for hf in range(2):
                            down(e - 1, i, hf)
                            yield 1
                if e <= NE:
                    gate_up(e, 1)
                    yield 1
                    if e + 2 <= NE:
                        load_gu(e + 2)
                if e >= 1:
                    for i in range(2, 4):
                        for hf in range(2):
                            down(e - 1, i, hf)
                            yield 1
                if e + 1 <= NE:
                    load_dn(e + 1)

        def moe_cast():
            for c in range(8):
                if c % 2 == 0:
                    A(I.copy(hT.t[:, c, :], big16[:, c, :]), BIGT.Ts, [hT.Ts[c]])
                else:
                    V(I.tensor_copy(hT.t[:, c, :], big16[:, c, :]), BIGT.Ts, [hT.Ts[c]])

        def acc_init(i):
            A(I.activation(ACC.t[:, i, :], R.t[:, i, :], AF.Copy, scale=float(ALPHA)), [R.Ts[i]], [ACC.Ts[i]])

        def stage_h(blk):
            fw.dma("sp", s_ln, lngb.t[:], lnp_d[2], writes=[lngb.T])
            for i in range(4):
                s = lnst4[i]
                xap = ACC.t[:, i, :]
                V(I.bn_stats(s.t[:, 0:6], xap[:, 0:512]), [ACC.Ts[i]], [s.T])
                V(I.bn_stats(s.t[:, 6:12], xap[:, 512:1024]), [ACC.Ts[i]], [s.T])
                V(I.bn_aggr(s.t[:, 12:14], s.t[:, 0:12]), [s.T], [s.T])
                A(I.activation(s.t[:, 14:15], s.t[:, 13:14], AF.Sqrt, bias=float(EPS)), [s.T], [s.T])
                V(I.reciprocal(s.t[:, 14:15], s.t[:, 14:15]), [s.T], [s.T])
            stg = [(xt[0].t[:], xt[0].T), (R.t[:, 0, :], R.Ts[0]), (R.t[:, 1, :], R.Ts[1]), (R.t[:, 2, :], R.Ts[2])]
            for i in range(4):
                s = lnst4[i]
                oap, oT = stg[i]
                V(I.tensor_scalar(oap, ACC.t[:, i, :], s.t[:, 12:13], s.t[:, 14:15], ALU.subtract, ALU.mult), [ACC.Ts[i], s.T], [oT])
                if blk + 1 < NB:
                    acc_init(i)
                V(I.tensor_tensor(oap, oap, lngb.t[:, 0:D], ALU.mult), [oT, lngb.T], [oT])
                G(I.tensor_tensor(oap, oap, lngb.t[:, D:2 * D], ALU.add), [oT, lngb.T], [oT])
                r0 = blk * TB + i * 128
                fw.dma("pool", s_y[i], y[r0:r0 + 128, :], oap, reads=[oT])

        def run_all(g):
            for _ in g:
                pass

        def interleave(gm, gf, per):
            acc = 0.0
            m_alive = True
            for _ in range(HEAD_START):
                next(gm)
            wtot = [0]
            for w in gf:
                wtot[0] += w
                acc += w * per
                while m_alive and acc >= 1.0:
                    try:
                        next(gm)
                    except StopIteration:
                        m_alive = False
                    acc -= 1.0
            left = 0
            if m_alive:
                for _ in gm:
                    left += 1
            if not hasattr(interleave, "_p"):
                interleave._p = True
                print("interleave: front weight total", wtot[0], "moe units left after front", left)

        run_all(gen_front(0))
        moe_cast()
        for i in range(4):
            acc_init(i)
        for blk in range(NB):
            gm = gen_moe(blk)
            if blk + 1 < NB:
                interleave(gm, gen_front(blk + 1), RATIO)
                moe_cast()
            else:
                run_all(gm)
            stage_h(blk)
        fw.barrier()
        fw.emit()
        print("ops:", fw.n_ops)
    return nc


def host_consts(router_bias):
    c = np.zeros((128, NCST), np.float32)
    c[:, C_ID:C_ID + 128] = np.eye(128, dtype=np.float32)
    p = np.arange(128)[:, None] % 64
    t = np.arange(64)[None, :]
    c[:, C_MASK:C_MASK + 64] = (p <= t).astype(np.float32)
    c[:, C_ONES:C_ONES + 128] = 1.0
    c[:, C_NH:C_NH + 1] = -0.5
    for h in range(4):
        c[h, C_SEL + h * 128:C_SEL + (h + 1) * 128] = 1.0
    c[:, C_RB:C_RB + NE] = np.asarray(router_bias, np.float32).reshape(1, NE)
    return c


def fm(v):
    v = np.asarray(v, np.float32)
    return np.ascontiguousarray(v.reshape(-1, 128).T)


def host_params(inp):
    p = np.zeros((128, NPRM), np.float32)
    p[:, P_LB0:P_LB0 + 8] = fm(inp["hg_lb_logits"][0])
    p[:, P_LB1:P_LB1 + 8] = fm(inp["hg_lb_logits"][1])
    p[:, P_HGG:P_HGG + 8] = fm(inp["hg_norm_g"][0])
    for tp in range(4):
        p[:, P_CW + tp * 8:P_CW + tp * 8 + 8] = fm(inp["ml_conv_w"][0, tp])
    p[:, P_CB:P_CB + 8] = fm(inp["ml_conv_b"][0])
    p[:, P_MLG:P_MLG + 8] = fm(inp["ml_norm_g"][0])
    p[0:4, P_IGB] = np.asarray(inp["ml_ig_bias"][0], np.float32)
    p[0:4, P_FGB] = np.asarray(inp["ml_fg_bias"][0], np.float32)
    return p


def host_shared(inp):
    f = lambda a: np.ascontiguousarray(np.asarray(a, np.float32))
    lnp = np.zeros((3, 128, 2 * D), np.float32)
    for k, (g, b) in enumerate((("ln_emb_g", "ln_emb_b"), ("ln1_g", "ln1_b"), ("ln2_g", "ln2_b"))):
        lnp[k, :, 0:D] = np.asarray(inp[g], np.float32).reshape(1, D)
        lnp[k, :, D:] = np.asarray(inp[b], np.float32).reshape(1, D)
    return {
        "w_in": f(inp["w_in"][0]), "w_a": f(inp["w_branch_a"][0]), "w_b": f(inp["w_branch_b"][0]),
        "w_o": f(inp["w_out"][0]), "w_r": f(inp["w_router"][0]),
        "w_g": np.concatenate([f(inp["w_exp_gate"][0]), f(inp["w_sh_gate"][0])[None]], 0),
        "w_u": np.concatenate([f(inp["w_exp_up"][0]), f(inp["w_sh_up"][0])[None]], 0),
        "w_d": np.concatenate([f(inp["w_exp_down"][0]), f(inp["w_sh_down"][0])[None]], 0),
        "cst": host_consts(inp["router_bias"][0]), "prm": host_params(inp), "lnp": lnp,
    }


def make_xin(x_b, meta, NB):
    xin = np.zeros((NB * TB, D), np.float32)
    xin[0:NMETA] = meta
    n = x_b.shape[0]
    xin[NMETA:NMETA + n] = x_b
    return xin


_NC_CACHE = {}


def kernel(**inputs):
    x = np.asarray(inputs["x"], np.float32)
    B, S, _ = x.shape
    NB = -(-(S + NMETA) // TB)
    shared = host_shared(inputs)
    meta = np.asarray(inputs["meta_tokens"], np.float32)
    if NB not in _NC_CACHE:
        _NC_CACHE[NB] = build(NB)
    nc = _NC_CACHE[NB]
    in_maps = []
    for b in range(B):
        m = dict(shared)
        m["xin"] = make_xin(x[b], meta, NB)
        in_maps.append(m)
    res = run_bass_kernel_spmd(nc, in_maps, core_ids=list(range(B)))
    out = np.stack([r["y"][NMETA:NMETA + S] for r in res.results], 0)
    return out.astype(np.float32)
```
